# Optimizing a Trainium2 kernel written in Bass

```python
import math
import jax
import jax.numpy as jnp
from jax import lax
import numpy as np

D_MODEL = 4096
BATCH = 2
SEQ = 8192
DEPTH = 2

CTX_LEN = 256
GRID_W = 64
HEAD_DIM = D_MODEL // 32
A_HEADS = 8
A_KV_HEADS = 2
A_WINDOW = 128
A_BLOCK = 128
B_HEADS = 8
NA_ROWS = 8
NA_COLS = 16
C_HEADS = 8
C_SUB_DIM = HEAD_DIM // 2
Q_BLOCK = 128
D_HEADS = 8
RET_CHUNK = 128
ROPE_BASE = 10000.0
N_GROUPS = 4
EXPERTS_PER_GROUP = 8
N_EXPERTS = N_GROUPS * EXPERTS_PER_GROUP
TOP_K_IN_GROUP = 2
D_EXPERT = D_MODEL // 8
MOE_BLOCK = 128
EPS = 1e-6
NEG_INF = -1e30

A_Q = A_HEADS * HEAD_DIM
A_KV = A_KV_HEADS * HEAD_DIM
B_W = B_HEADS * HEAD_DIM
C_W = C_HEADS * HEAD_DIM
D_W = D_HEADS * HEAD_DIM
MIX_WIDTH = A_Q + B_W + C_W + D_W
IN_WIDTHS = [A_Q, A_KV, A_KV, B_W, B_W, B_W, C_W, C_W, C_W, D_W, D_W, D_W, D_W]
IN_COLS = sum(IN_WIDTHS)
IN_OFFSETS = [int(v) for v in np.cumsum(IN_WIDTHS)[:-1]]

kernel_name = 'hybrid_headgroup_dit_moe_trunk'


def rms_norm(x, gain=None):
    xf = x.astype(jnp.float32)
    y = xf * lax.rsqrt(jnp.mean(xf * xf, axis=-1, keepdims=True) + EPS)
    if gain is not None:
        y = y * gain.astype(jnp.float32)
    return y.astype(x.dtype)


def modulate(x, gain, shift, scale):
    return rms_norm(x, gain) * (1.0 + scale) + shift


def rope_tables(L, dim):
    t = jnp.arange(L)
    row = (t // GRID_W).astype(jnp.float32)
    col = (t % GRID_W).astype(jnp.float32)
    quarter = dim // 4
    inv = ROPE_BASE ** (-jnp.arange(quarter, dtype=jnp.float32) / quarter)
    ar = row[:, None] * inv[None, :]
    ac = col[:, None] * inv[None, :]
    ang = jnp.concatenate([ar, ar, ac, ac], axis=-1)
    return jnp.cos(ang), jnp.sin(ang)


def apply_rope(x, cos, sin):
    half = x.shape[-1] // 2
    quarter = half // 2
    xr, xcol = x[..., :half], x[..., half:]
    rot = jnp.concatenate([-xr[..., quarter:], xr[..., :quarter], -xcol[..., quarter:], xcol[..., :quarter]], axis=-1)
    return (x.astype(jnp.float32) * cos + rot.astype(jnp.float32) * sin).astype(x.dtype)


def ctx_attention(q, k, v, sink):
    B_, C, H, dh = q.shape
    KV = k.shape[2]
    G = H // KV
    qg = q.reshape(B_, C, KV, G, dh)
    s = jnp.einsum('bqkgd,bckd->bkgqc', qg, k).astype(jnp.float32) * (dh ** -0.5)
    if sink is not None:
        s_sink = jnp.broadcast_to(sink.astype(jnp.float32).reshape(1, KV, G, 1, 1), s.shape[:-1] + (1,))
        s = jnp.concatenate([s, s_sink], axis=-1)
    p = jax.nn.softmax(s, axis=-1)[..., :C].astype(v.dtype)
    return jnp.einsum('bkgqc,bckd->bqkgd', p, v).reshape(B_, C, H * dh)


def band_blocks(t, blk):
    B_, L = t.shape[0], t.shape[1]
    nb = L // blk
    tp = jnp.pad(t, ((0, 0), (blk, blk), (0, 0), (0, 0)))
    tb = tp.reshape(B_, nb + 2, blk, t.shape[2], t.shape[3])
    return jnp.concatenate([tb[:, :-2], tb[:, 1:-1], tb[:, 2:]], axis=2)


def window_mask(nb, blk, window):
    s = jnp.arange(blk)[:, None]
    t = jnp.arange(3 * blk)[None, :]
    rel = t - blk - s
    j = jnp.arange(nb)[:, None, None] * blk - blk + t[None]
    return (jnp.abs(rel) <= window)[None] & (j >= 0) & (j < nb * blk)


def window_gqa_latent(q, k, v, kc, vc, sink):
    B_, L, H, dh = q.shape
    KV = k.shape[2]
    G = H // KV
    nb = L // A_BLOCK
    C = kc.shape[1]
    scale = dh ** -0.5
    qb = q.reshape(B_, nb, A_BLOCK, KV, G, dh)
    kb = band_blocks(k, A_BLOCK)
    vb = band_blocks(v, A_BLOCK)
    s_lat = jnp.einsum('bnqkgd,bntkd->bnkgqt', qb, kb).astype(jnp.float32) * scale
    s_lat = jnp.where(window_mask(nb, A_BLOCK, A_WINDOW)[None, :, None, None], s_lat, NEG_INF)
    s_ctx = jnp.einsum('bnqkgd,bckd->bnkgqc', qb, kc).astype(jnp.float32) * scale
    s_sink = jnp.broadcast_to(sink.astype(jnp.float32).reshape(1, 1, KV, G, 1, 1), s_lat.shape[:-1] + (1,))
    p = jax.nn.softmax(jnp.concatenate([s_lat, s_ctx, s_sink], axis=-1), axis=-1).astype(v.dtype)
    nt = 3 * A_BLOCK
    o = (jnp.einsum('bnkgqt,bntkd->bnqkgd', p[..., :nt], vb)
         + jnp.einsum('bnkgqc,bckd->bnqkgd', p[..., nt:nt + C], vc))
    return o.reshape(B_, L, H * dh)


def neighbourhood_latent(q, k, v, kc, vc, rpb):
    B_, L, H, dh = q.shape
    W = GRID_W
    R = L // W
    KH = min(NA_ROWS, R)
    KW = min(NA_COLS, W)
    scale = dh ** -0.5
    r = jnp.arange(R)
    rs = jnp.clip(r - KH // 2, 0, R - KH)
    row_idx = rs[:, None] + jnp.arange(KH)[None, :]
    qg = q.reshape(B_, R, W, H, dh)
    kg = k.reshape(B_, R, W, H, dh)[:, row_idx].reshape(B_, R, KH * W, H, dh)
    vg = v.reshape(B_, R, W, H, dh)[:, row_idx].reshape(B_, R, KH * W, H, dh)
    cidx = jnp.arange(W)
    cs = jnp.clip(cidx - KW // 2, 0, W - KW)
    colmask = (cidx[None, :] >= cs[:, None]) & (cidx[None, :] < cs[:, None] + KW)
    rel_r = row_idx - r[:, None] + (NA_ROWS - 1)
    rel_c = jnp.clip(cidx[None, :] - cidx[:, None] + (NA_COLS - 1), 0, 2 * NA_COLS - 2)
    bias = rpb.astype(jnp.float32)[:, rel_r[:, None, :, None], rel_c[None, :, None, :]]
    bias = jnp.where(colmask[None, None, :, None, :], bias, NEG_INF)
    bias = jnp.transpose(bias, (1, 0, 2, 3, 4)).reshape(R, H, W, KH * W)
    s_lat = jnp.einsum('brqhd,brkhd->brhqk', qg, kg).astype(jnp.float32) * scale + bias[None]
    s_ctx = jnp.einsum('brqhd,bchd->brhqc', qg, kc).astype(jnp.float32) * scale
    p = jax.nn.softmax(jnp.concatenate([s_lat, s_ctx], axis=-1), axis=-1).astype(v.dtype)
    nk = KH * W
    o = (jnp.einsum('brhqk,brkhd->brqhd', p[..., :nk], vg)
         + jnp.einsum('brhqc,bchd->brqhd', p[..., nk:], vc))
    return o.reshape(B_, L, H * dh)


def diff_lambda(lambda_params, lam_init):
    lp = lambda_params.astype(jnp.float32)
    return jnp.exp(jnp.sum(lp[0] * lp[1])) - jnp.exp(jnp.sum(lp[2] * lp[3])) + lam_init


def diff_attend(q, k, v, lam):
    s = jnp.einsum('bqhsd,bkhsd->bhsqk', q, k).astype(jnp.float32) * (q.shape[-1] ** -0.5)
    p = jax.nn.softmax(s, axis=-1)
    a = p[:, :, 0] - lam * p[:, :, 1]
    return jnp.einsum('bhqk,bkhe->bqhe', a.astype(v.dtype), v)


def diff_finish(o, gain, lam_init):
    y = rms_norm(o, gain) * (1.0 - lam_init)
    return y.reshape(o.shape[0], o.shape[1], o.shape[2] * o.shape[3])


def diff_attention_latent(q, k, v, kc, vc, lam, gain, lam_init):
    B_, L = q.shape[0], q.shape[1]
    nb = L // Q_BLOCK
    k_all = jnp.concatenate([k, kc], axis=1)
    v_all = jnp.concatenate([v, vc], axis=1)
    qb = jnp.moveaxis(q.reshape(B_, nb, Q_BLOCK, q.shape[2], q.shape[3], q.shape[4]), 1, 0)
    ob = lax.map(lambda blk: diff_attend(blk, k_all, v_all, lam), qb)
    o = jnp.moveaxis(ob, 0, 1).reshape(B_, L, ob.shape[3], ob.shape[4])
    return diff_finish(o, gain, lam_init)


def retention_scan(q, k, v, log_gamma, state0, with_out):
    B_, L, H, dk = q.shape
    dv = v.shape[-1]
    n = L // RET_CHUNK
    pos = jnp.arange(RET_CHUNK, dtype=jnp.float32)
    rel = pos[:, None] - pos[None, :]
    intra = jnp.where(rel[None] >= 0, jnp.exp(log_gamma[:, None, None] * jnp.maximum(rel, 0.0)[None]), 0.0)
    q_decay = jnp.exp(log_gamma[None, :] * (pos[:, None] + 1.0))
    k_decay = jnp.exp(log_gamma[None, :] * (RET_CHUNK - 1.0 - pos[:, None]))
    chunk_decay = jnp.exp(log_gamma * RET_CHUNK)[None, :, None, None]

    def chunks(t):
        return jnp.moveaxis(t.reshape(B_, n, RET_CHUNK, H, t.shape[-1]), 1, 0)

    def step(S, inp):
        qb, kb, vb = inp
        S_new = S * chunk_decay + jnp.einsum('bjhd,bjhe->bhde', kb * k_decay[None, :, :, None], vb)
        if not with_out:
            return S_new, None
        s = jnp.einsum('bihd,bjhd->bhij', qb, kb) * intra[None]
        o = (jnp.einsum('bhij,bjhe->bihe', s, vb)
             + jnp.einsum('bihd,bhde->bihe', qb, S) * q_decay[None, :, :, None])
        return S_new, o

    S_fin, o = lax.scan(step, state0, (chunks(q), chunks(k), chunks(v)))
    if with_out:
        o = jnp.moveaxis(o, 0, 1).reshape(B_, L, H, dv)
    return o, S_fin


def retention_mixer(q, k, v, g, qc, kc, vc, gc, log_decay, with_ctx):
    B_, L, H, dk = q.shape
    dv = v.shape[-1]
    f32 = lambda t: t.astype(jnp.float32)
    scale = dk ** -0.5
    qf, kf, vf = f32(q) * scale, f32(k), f32(v)
    qcf, kcf, vcf = f32(qc) * scale, f32(kc), f32(vc)
    state0 = jnp.zeros((B_, H, dk, dv), jnp.float32)
    outs, outs_c = [], []
    for direction in range(2):
        lg = log_decay[direction].astype(jnp.float32)
        if direction == 0:
            rev = lambda t: t
        else:
            rev = lambda t: jnp.flip(t, axis=1)
        oc, s_ctx = retention_scan(rev(qcf), rev(kcf), rev(vcf), lg, state0, with_ctx)
        ol, _ = retention_scan(rev(qf), rev(kf), rev(vf), lg, s_ctx, True)
        outs.append(rev(ol))
        if with_ctx:
            outs_c.append(rev(oc))
    y = rms_norm(outs[0] + outs[1]) * jax.nn.silu(f32(g).reshape(B_, L, H, dv))
    y = y.reshape(B_, L, H * dv).astype(g.dtype)
    yc = None
    if with_ctx:
        C = qc.shape[1]
        yc = rms_norm(outs_c[0] + outs_c[1]) * jax.nn.silu(f32(gc).reshape(B_, C, H, dv))
        yc = yc.reshape(B_, C, H * dv).astype(gc.dtype)
    return y, yc


def hier_moe(h, router_group, router_expert, w_gate, w_up, w_down):
    T, D = h.shape
    hf = h.astype(jnp.float32)
    g_prob = jax.nn.softmax(hf @ router_group.astype(jnp.float32), axis=-1)
    g_val, g_idx = lax.top_k(g_prob, 1)
    e_logits = (hf @ router_expert.astype(jnp.float32)).reshape(T, N_GROUPS, EXPERTS_PER_GROUP)
    sel = jnp.take_along_axis(e_logits, g_idx[:, :, None], axis=1)[:, 0]
    e_val, e_idx = lax.top_k(sel, TOP_K_IN_GROUP)
    weights = g_val * jax.nn.softmax(e_val, axis=-1)
    expert_id = g_idx * EXPERTS_PER_GROUP + e_idx
    A = T * TOP_K_IN_GROUP
    flat_e = expert_id.reshape(A)
    flat_tok = jnp.arange(A) // TOP_K_IN_GROUP
    flat_w = weights.reshape(A)
    order = jnp.argsort(flat_e, stable=True)
    se = flat_e[order]
    counts = jnp.bincount(flat_e, length=N_EXPERTS)
    starts = jnp.cumsum(counts) - counts
    pcounts = (counts + MOE_BLOCK - 1) // MOE_BLOCK * MOE_BLOCK
    pends = jnp.cumsum(pcounts)
    pstarts = pends - pcounts
    dest = pstarts[se] + (jnp.arange(A) - starts[se])
    NB = (A + N_EXPERTS * (MOE_BLOCK - 1)) // MOE_BLOCK
    P = NB * MOE_BLOCK
    row_tok = jnp.full((P,), T, jnp.int32).at[dest].set(flat_tok[order].astype(jnp.int32))
    row_w = jnp.zeros((P,), jnp.float32).at[dest].set(flat_w[order])
    block_e = jnp.clip(jnp.searchsorted(pends, jnp.arange(NB) * MOE_BLOCK, side='right'), 0, N_EXPERTS - 1)
    hpad = jnp.concatenate([h, jnp.zeros((1, D), h.dtype)], axis=0)

    def expert_block(args):
        tok, e = args
        xb = hpad[tok]
        a = xb @ w_gate[e]
        u = xb @ w_up[e]
        return (jax.nn.silu(a) * u) @ w_down[e]

    yb = lax.map(expert_block, (row_tok.reshape(NB, MOE_BLOCK), block_e))
    y = yb.reshape(P, D) * row_w[:, None].astype(h.dtype)
    out = jnp.zeros((T + 1, D), h.dtype).at[row_tok].add(y)
    return out[:T]


def trunk_layer(x, xc, c, c_ctx, layer_idx, with_ctx, rope_a, rope_c,
                w_mod, b_mod, norm_mix, norm_ffn, w_in, w_out,
                qk_norm_a, sink_a, qk_norm_b, rpb_b, qk_norm_c, lambda_c, subln_c,
                ret_log_decay, router_group, router_expert, w_gate, w_up, w_down):
    B_, L, D = x.shape
    C = xc.shape[1]
    mod = jax.nn.silu(c) @ w_mod + b_mod
    mod_c = jax.nn.silu(c_ctx) @ w_mod + b_mod
    sh1, sc1, g1, sh2, sc2, g2 = jnp.split(mod[:, None, :], 6, axis=-1)
    csh1, csc1, cg1, csh2, csc2, cg2 = jnp.split(mod_c, 6, axis=-1)

    h = modulate(x, norm_mix, sh1, sc1)
    hc = modulate(xc, norm_mix, csh1, csc1)
    (aq, ak, av, bq, bk, bv, cq, ck, cv, dq, dk, dv, dg) = jnp.split(h @ w_in, IN_OFFSETS, axis=-1)
    (aqc, akc, avc, bqc, bkc, bvc, cqc, ckc, cvc, dqc, dkc, dvc, dgc) = jnp.split(hc @ w_in, IN_OFFSETS, axis=-1)
    heads = lambda t, n: t.reshape(t.shape[0], t.shape[1], n, -1)
    subheads = lambda t: t.reshape(t.shape[0], t.shape[1], C_HEADS, 2, C_SUB_DIM)

    cos_a, sin_a = rope_a
    qa = apply_rope(rms_norm(heads(aq, A_HEADS), qk_norm_a[0]), cos_a[:, None], sin_a[:, None])
    ka = apply_rope(rms_norm(heads(ak, A_KV_HEADS), qk_norm_a[1]), cos_a[:, None], sin_a[:, None])
    kac = rms_norm(heads(akc, A_KV_HEADS), qk_norm_a[1])
    vac = heads(avc, A_KV_HEADS)
    out_a = window_gqa_latent(qa, ka, heads(av, A_KV_HEADS), kac, vac, sink_a)

    kbc = rms_norm(heads(bkc, B_HEADS), qk_norm_b[1])
    vbc = heads(bvc, B_HEADS)
    out_b = neighbourhood_latent(rms_norm(heads(bq, B_HEADS), qk_norm_b[0]),
                                 rms_norm(heads(bk, B_HEADS), qk_norm_b[1]),
                                 heads(bv, B_HEADS), kbc, vbc, rpb_b)

    lam_init = 0.8 - 0.6 * math.exp(-0.3 * layer_idx)
    lam = diff_lambda(lambda_c, lam_init)
    cos_c, sin_c = rope_c
    qcl = apply_rope(rms_norm(subheads(cq), qk_norm_c[0]), cos_c[:, None, None], sin_c[:, None, None])
    kcl = apply_rope(rms_norm(subheads(ck), qk_norm_c[1]), cos_c[:, None, None], sin_c[:, None, None])
    kcc = rms_norm(subheads(ckc), qk_norm_c[1])
    vcc = heads(cvc, C_HEADS)
    out_c = diff_attention_latent(qcl, kcl, heads(cv, C_HEADS), kcc, vcc, lam, subln_c, lam_init)

    out_d, out_dc = retention_mixer(heads(dq, D_HEADS), heads(dk, D_HEADS), heads(dv, D_HEADS), dg,
                                    heads(dqc, D_HEADS), heads(dkc, D_HEADS), heads(dvc, D_HEADS), dgc,
                                    ret_log_decay, with_ctx)

    x = x + g1 * (jnp.concatenate([out_a, out_b, out_c, out_d], axis=-1) @ w_out)
    if with_ctx:
        out_ac = ctx_attention(rms_norm(heads(aqc, A_HEADS), qk_norm_a[0]), kac, vac, sink_a)
        out_bc = ctx_attention(rms_norm(heads(bqc, B_HEADS), qk_norm_b[0]), kbc, vbc, None)
        out_cc = diff_finish(diff_attend(rms_norm(subheads(cqc), qk_norm_c[0]), kcc, vcc, lam), subln_c, lam_init)
        xc = xc + cg1 * (jnp.concatenate([out_ac, out_bc, out_cc, out_dc], axis=-1) @ w_out)

    h2 = modulate(x, norm_ffn, sh2, sc2).reshape(B_ * L, D)
    if with_ctx:
        h2c = modulate(xc, norm_ffn, csh2, csc2).reshape(B_ * C, D)
        y2 = hier_moe(jnp.concatenate([h2, h2c], axis=0), router_group, router_expert, w_gate, w_up, w_down)
        x = x + g2 * y2[:B_ * L].reshape(B_, L, D)
        xc = xc + cg2 * y2[B_ * L:].reshape(B_, C, D)
    else:
        x = x + g2 * hier_moe(h2, router_group, router_expert, w_gate, w_up, w_down).reshape(B_, L, D)
    return x, xc


def setup_inputs(seed: int = 0) -> dict:
    key = jax.random.key(seed)
    ks = jax.random.split(key, 24)
    D = D_MODEL
    nrm = lambda k, shape, s: jax.random.normal(k, shape, jnp.float32) * s
    base_decay = jnp.asarray(np.log(1.0 - 2.0 ** (-5.0 - np.arange(D_HEADS))).astype(np.float32))
    return {
        'x': nrm(ks[0], (BATCH, SEQ, D), 1.0),
        'c': nrm(ks[1], (BATCH, D), 1.0),
        'ctx': nrm(ks[2], (BATCH, CTX_LEN, D), 1.0),
        'c_ctx': nrm(ks[3], (D,), 1.0),
        'w_mod': nrm(ks[4], (DEPTH, D, 6 * D), 0.5 * D ** -0.5),
        'b_mod': nrm(ks[5], (DEPTH, 6 * D), 0.02),
        'norm_mix': 1.0 + nrm(ks[6], (DEPTH, D), 0.02),
        'norm_ffn': 1.0 + nrm(ks[7], (DEPTH, D), 0.02),
        'w_in': nrm(ks[8], (DEPTH, D, IN_COLS), D ** -0.5),
        'w_out': nrm(ks[9], (DEPTH, MIX_WIDTH, D), MIX_WIDTH ** -0.5),
        'qk_norm_a': 1.0 + nrm(ks[10], (DEPTH, 2, HEAD_DIM), 0.02),
        'sink_a': nrm(ks[11], (DEPTH, A_HEADS), 0.5),
        'qk_norm_b': 1.0 + nrm(ks[12], (DEPTH, 2, HEAD_DIM), 0.02),
        'rpb_b': nrm(ks[13], (DEPTH, B_HEADS, 2 * NA_ROWS - 1, 2 * NA_COLS - 1), 0.1),
        'qk_norm_c': 1.0 + nrm(ks[14], (DEPTH, 2, C_SUB_DIM), 0.02),
        'lambda_c': nrm(ks[15], (DEPTH, 4, C_SUB_DIM), 0.1),
        'subln_c': 1.0 + nrm(ks[16], (DEPTH, HEAD_DIM), 0.02),
        'ret_log_decay': base_decay[None, None, :] * (1.0 + nrm(ks[17], (DEPTH, 2, D_HEADS), 0.05)),
        'router_group': nrm(ks[18], (DEPTH, D, N_GROUPS), D ** -0.5),
        'router_expert': nrm(ks[19], (DEPTH, D, N_EXPERTS), D ** -0.5),
        'w_gate': nrm(ks[20], (DEPTH, N_EXPERTS, D, D_EXPERT), D ** -0.5),
        'w_up': nrm(ks[21], (DEPTH, N_EXPERTS, D, D_EXPERT), D ** -0.5),
        'w_down': nrm(ks[22], (DEPTH, N_EXPERTS, D_EXPERT, D), D_EXPERT ** -0.5),
    }


def reference(x, c, ctx, c_ctx, w_mod, b_mod, norm_mix, norm_ffn, w_in, w_out,
              qk_norm_a, sink_a, qk_norm_b, rpb_b, qk_norm_c, lambda_c, subln_c,
              ret_log_decay, router_group, router_expert, w_gate, w_up, w_down):
    L = x.shape[1]
    rope_a = rope_tables(L, HEAD_DIM)
    rope_c = rope_tables(L, C_SUB_DIM)
    xc = ctx
    for l in range(DEPTH):
        x, xc = trunk_layer(x, xc, c, c_ctx, l, l < DEPTH - 1, rope_a, rope_c,
                            w_mod[l], b_mod[l], norm_mix[l], norm_ffn[l], w_in[l], w_out[l],
                            qk_norm_a[l], sink_a[l], qk_norm_b[l], rpb_b[l], qk_norm_c[l], lambda_c[l],
                            subln_c[l], ret_log_decay[l], router_group[l], router_expert[l],
                            w_gate[l], w_up[l], w_down[l])
    return x
```

```python
import numpy as np
import concourse.bass as bass
import concourse.mybir as mybir
from concourse.bass_utils import run_bass_kernel_spmd

F32 = mybir.dt.float32
BF16 = mybir.dt.bfloat16
I32 = mybir.dt.int32
ALU = mybir.AluOpType
AF = mybir.ActivationFunctionType
AX = mybir.AxisListType


class Sched:
    COMPUTE = ("pe", "act", "dve", "pool")

    def __init__(self, nc, n_dma_sems=12):
        self.nc = nc
        self.ops = []
        self.last_w = {}
        self.readers = {}
        self.n_dma_sems = n_dma_sems
        self.dma_count = {"sp": 0, "pool": 0, "act": 0}
        self.bar_idx = None

    def _add(self, eng, fn, reads, writes, is_dma):
        idx = len(self.ops)
        deps = set()
        if self.bar_idx is not None:
            deps.add(self.bar_idx)
        for k in reads:
            w = self.last_w.get(k)
            if w is not None:
                deps.add(w)
        for k in writes:
            w = self.last_w.get(k)
            if w is not None:
                deps.add(w)
            for r in self.readers.get(k, ()):
                deps.add(r)
        for k in writes:
            self.last_w[k] = idx
            self.readers[k] = []
        for k in reads:
            self.readers.setdefault(k, []).append(idx)
        op = dict(eng=eng, fn=fn, deps=deps, dma=is_dma, sig=False)
        if is_dma:
            j = self.dma_count[eng]
            self.dma_count[eng] += 1
            op["dma_j"] = j
        self.ops.append(op)
        return idx

    def op(self, eng, fn, reads=(), writes=()):
        return self._add(eng, fn, tuple(reads), tuple(writes), False)

    def dma(self, q, out, in_, reads=(), writes=(), **kw):
        return self._add(q, lambda e: e.dma_start(out=out, in_=in_, **kw), tuple(reads), tuple(writes), True)

    def dma_fn(self, q, fn, reads=(), writes=()):
        return self._add(q, fn, tuple(reads), tuple(writes), True)

    def emit(self):
        nc = self.nc
        ops = self.ops
        N = self.n_dma_sems
        for i, op in enumerate(ops):
            best = {}
            dl = []
            for d in op["deps"]:
                o = ops[d]
                if o["dma"]:
                    dl.append(d)
                else:
                    if o["eng"] == op["eng"] and not op["dma"] and op["eng"] == "pe":
                        continue
                    if o["eng"] not in best or best[o["eng"]] < d:
                        best[o["eng"]] = d
            op["cdeps"] = best
            op["ddeps"] = dl
            for d in best.values():
                ops[d]["sig"] = True
        cnt = {e: 0 for e in self.COMPUTE}
        for op in ops:
            if not op["dma"] and op["sig"]:
                cnt[op["eng"]] += 1
                op["cnt"] = cnt[op["eng"]]
        import contextlib
        with contextlib.ExitStack() as st:
            csem = {e: st.enter_context(nc.semaphore("s_" + e)) for e in self.COMPUTE}
            dsem = {}
            for q, c in self.dma_count.items():
                if c > 0:
                    dsem[q] = [st.enter_context(nc.semaphore("d_%s%d" % (q, i))) for i in range(min(N, c))]
            block = st.enter_context(nc.Block())
            streams = {}
            for i, op in enumerate(ops):
                streams.setdefault(op["eng"], []).append(i)
            engmap = {"pe": "tensor", "act": "scalar", "dve": "vector", "pool": "gpsimd", "sp": "sync"}

            def run_stream(ename, e):
                waited = {}

                def wait(sem, val, key):
                    if waited.get(key, 0) >= val:
                        return
                    waited[key] = val
                    e.wait_ge(sem, val)

                for i in streams.get(ename, []):
                    op = ops[i]
                    for de, d in op["cdeps"].items():
                        wait(csem[de], ops[d]["cnt"], ("c", de))
                    for d in op["ddeps"]:
                        o = ops[d]
                        j = o["dma_j"]
                        wait(dsem[o["eng"]][j % N], 16 * (j // N + 1), ("d", o["eng"], j % N))
                    if op["dma"]:
                        j = op["dma_j"]
                        if j >= N:
                            wait(dsem[ename][j % N], 16 * (j // N), ("d", ename, j % N))
                        ins = op["fn"](e)
                        ins.then_inc(dsem[ename][j % N], 16)
                    else:
                        ins = op["fn"](e)
                        if op["sig"]:
                            ins.then_inc(csem[ename], 1)
                if ename == "sp":
                    for q, c in self.dma_count.items():
                        for s in range(min(N, c)):
                            last_j = ((c - 1 - s) // N) * N + s
                            wait(dsem[q][s], 16 * (last_j // N + 1), ("d", q, s))
                    for ce in self.COMPUTE:
                        if cnt[ce] > 0:
                            wait(csem[ce], cnt[ce], ("c", ce))

            for ename in ("sp", "pe", "act", "dve", "pool"):
                if ename in streams or ename == "sp":
                    getattr(block, engmap[ename])(lambda e, en=ename: run_stream(en, e))

    def barrier(self):
        keys = list(self.last_w.keys()) + list(self.readers.keys())
        self.bar_idx = self.op("dve", lambda e: e.engine_nop() if hasattr(e, "engine_nop") else e.nop(), reads=keys, writes=["__bar__"] + keys)


D = 4096
HD = 128
DEPTH = 2
CTX = 256
GRID_W = 64
EPS = 1e-6
NEG = -30000.0
NCORES = 8
import contextlib
import ml_dtypes
NPBF = ml_dtypes.bfloat16


def _run(nc, in_maps):
    res = run_bass_kernel_spmd(nc, in_maps, core_ids=list(range(NCORES)))
    return res.results


class Ctx:
    def __init__(self):
        self.nc = bass.Bass("TRN2", target_bir_lowering=False)
        self.st = contextlib.ExitStack()
        self.S = Sched(self.nc)
        self.ps = None

    def din(self, name, shape, dt):
        return self.nc.dram_tensor(name, list(shape), dt, kind="ExternalInput").ap()

    def dout(self, name, shape, dt):
        return self.nc.dram_tensor(name, list(shape), dt, kind="ExternalOutput").ap()

    def dscr(self, name, shape, dt):
        return self.nc.dram_tensor(name, list(shape), dt).ap()

    def sb(self, name, shape, dt):
        return self.st.enter_context(self.nc.sbuf_tensor(name, list(shape), dt))[:]

    def psum(self):
        if self.ps is None:
            self.ps = [self.st.enter_context(self.nc.psum_tensor("ps%d" % i, [128, 512], F32))[:] for i in range(8)]
        return self.ps

    def finish(self):
        self.S.emit()
        self.st.close()
        return self.nc


def emit_modnorm(S, x_ap, P, gs_ap, sh_ap, out_ap, tmp_f, junk_bf, st_ap, kx, kgs, ksh, kout, ktmp, kjunk, kst, eng2="pool"):
    S.op("act", lambda e: e.activation(out=junk_bf, in_=x_ap, func=AF.Square, accum_out=st_ap[:, 0:1]),
         reads=kx, writes=kjunk + kst)
    S.op("dve", lambda e: e.tensor_scalar(st_ap[:, 1:2], st_ap[:, 0:1], 1.0 / D, EPS, ALU.mult, ALU.add),
         reads=kst, writes=kst)
    S.op("act", lambda e: e.activation(out=st_ap[:, 2:3], in_=st_ap[:, 1:2], func=AF.Sqrt), reads=kst, writes=kst)
    S.op("dve", lambda e: e.reciprocal(st_ap[:, 3:4], st_ap[:, 2:3]), reads=kst, writes=kst)
    S.op("dve", lambda e: e.scalar_tensor_tensor(tmp_f, x_ap, st_ap[:, 3:4], gs_ap, ALU.mult, ALU.mult),
         reads=kx + kst + kgs, writes=ktmp)
    S.op(eng2, lambda e: e.tensor_tensor(out_ap, tmp_f, sh_ap, ALU.add), reads=ktmp + ksh, writes=kout)


def build_mod():
    cx = Ctx()
    S = cx.S
    CW = 6 * D // NCORES
    cinT = cx.din("cinT", [D, 3], F32)
    wm = cx.din("wm", [DEPTH, D, CW], F32)
    bm = cx.din("bm", [DEPTH, 3, CW], F32)
    out = cx.dout("mod", [DEPTH, 3, CW], F32)
    ps = cx.psum()
    ct = cx.sb("ct", [128, 32, 3], F32)
    wt = [cx.sb("wt%d" % i, [128, 32, 512], F32) for i in range(2)]
    bt = cx.sb("bt", [3, DEPTH, CW], F32)
    ot = cx.sb("ot", [3, DEPTH, CW], F32)
    S.dma("sp", ct, cinT.rearrange("(c p) r -> p c r", p=128), writes=["ct"])
    S.dma("sp", bt, bm.rearrange("l r c -> r l c"), writes=["bt"])
    S.op("act", lambda e: e.activation(out=ct, in_=ct, func=AF.Silu), reads=["ct"], writes=["ct"])
    n = 0
    for l in range(DEPTH):
        wv = wm[l].rearrange("(c p) n -> p c n", p=128)
        for s in range(CW // 512):
            w = wt[n % 2]
            kw = "wt%d" % (n % 2)
            for h in range(8):
                S.dma("sp" if h % 2 == 0 else "act", w[:, h * 4:(h + 1) * 4, :], wv[:, h * 4:(h + 1) * 4, s * 512:(s + 1) * 512],
                      writes=[kw + "_%d" % h])
            p = ps[n % 4]
            kp = "ps%d" % (n % 4)
            for k in range(32):
                S.op("pe", lambda e, p=p, w=w, k=k: e.matmul(p[0:3, :], ct[:, k, :], w[:, k, :], start=(k == 0), stop=(k == 31)),
                     reads=["ct", kw + "_%d" % (k // 4)], writes=[kp])
            S.op("dve", lambda e, p=p, l=l, s=s: e.tensor_tensor(ot[:, l, s * 512:(s + 1) * 512], p[0:3, :], bt[:, l, s * 512:(s + 1) * 512], ALU.add),
                 reads=[kp, "bt"], writes=["ot"])
            n += 1
    S.dma("sp", out.rearrange("l r c -> r l c"), ot, reads=["ot"])
    return cx.finish()


def run_mod(c, c_ctx, w_mod, b_mod):
    CW = 6 * D // NCORES
    cinT = np.ascontiguousarray(np.concatenate([c, c_ctx[None]], 0).T)
    nc = build_mod()
    ims = []
    for i in range(NCORES):
        sl = slice(i * CW, (i + 1) * CW)
        ims.append({"cinT": cinT, "wm": np.ascontiguousarray(w_mod[:, :, sl]),
                    "bm": np.ascontiguousarray(np.broadcast_to(b_mod[:, None, sl], (DEPTH, 3, CW)))})
    res = _run(nc, ims)
    return np.concatenate([r["mod"] for r in res], axis=2)


def load_bcast(S, q, dst, src_row, key, also=()):
    for h in range(4):
        S.dma(q, dst[:, h * 1024:(h + 1) * 1024], src_row[h * 1024:(h + 1) * 1024].partition_broadcast(128), writes=[key + "_%d" % h] + list(also))


BK4 = lambda k: [k + "_%d" % h for h in range(4)]


def build_D(TOKL, TOKC, do_combine, do_norm):
    cx = Ctx()
    S = cx.S
    TOK = TOKL + TOKC
    x = cx.din("x", [TOK, D], F32)
    if do_combine:
        ya = cx.din("ya", [TOK, D], BF16)
        yb = cx.din("yb", [TOK, D], BF16)
        wts = cx.din("wts", [TOK, 2], F32)
        g2 = cx.din("g2", [2, D], F32)
        xo = cx.dout("xo", [TOK, D], F32)
    if do_norm:
        nm = cx.din("nm", [D], F32)
        sc = cx.din("sc", [2, D], F32)
        sh = cx.din("sh", [2, D], F32)
        ho = cx.dout("h", [TOK, D], BF16)
    xt = [cx.sb("xt%d" % i, [128, D], F32) for i in range(2)]
    tmp = cx.sb("tmp", [128, D], F32)
    if do_combine:
        yat = [cx.sb("yat%d" % i, [128, D], BF16) for i in range(2)]
        ybt = [cx.sb("ybt%d" % i, [128, D], BF16) for i in range(2)]
        wt = [cx.sb("wtt%d" % i, [128, 2], F32) for i in range(2)]
        g2b = cx.sb("g2b", [128, D], F32)
    if do_norm:
        gsb = cx.sb("gsb", [128, D], F32)
        shb = cx.sb("shb", [128, D], F32)
        nmb = cx.sb("nmb", [128, D], F32)
        hb = [cx.sb("hb%d" % i, [128, D], BF16) for i in range(2)]
        junk = cx.sb("junk", [128, D], BF16)
        stt = [cx.sb("stt%d" % i, [128, 4], F32) for i in range(2)]
        load_bcast(S, "act", nmb, nm, "nmb")
    n = 0
    for seg in range(2):
        rows = TOKL if seg == 0 else TOKC
        base = 0 if seg == 0 else TOKL
        if rows == 0:
            continue
        if do_combine:
            load_bcast(S, "act", g2b, g2[seg], "g2b")
        if do_norm:
            load_bcast(S, "act", gsb, sc[seg], "gsb")
            load_bcast(S, "act", shb, sh[seg], "shb")
            S.op("dve", lambda e: e.scalar_tensor_tensor(gsb, gsb, 1.0, nmb, ALU.add, ALU.mult),
                 reads=BK4("gsb") + BK4("nmb"), writes=BK4("gsb"))
        r0 = 0
        while r0 < rows:
            P = min(128, rows - r0)
            i = n % 2
            xa = xt[i][0:P, :]
            kx = "xt%d" % i
            for h in range(2):
                S.dma("sp", xt[i][0:P, h * 2048:(h + 1) * 2048], x[base + r0:base + r0 + P, h * 2048:(h + 1) * 2048], writes=[kx])
            if do_combine:
                S.dma("sp", yat[i][0:P, :], ya[base + r0:base + r0 + P, :], writes=["ya%d" % i])
                S.dma("sp", ybt[i][0:P, :], yb[base + r0:base + r0 + P, :], writes=["yb%d" % i])
                S.dma("sp", wt[i][0:P, :], wts[base + r0:base + r0 + P, :], writes=["wt%d" % i])
                S.op("dve", lambda e, i=i, P=P: e.tensor_scalar(tmp[0:P, :], yat[i][0:P, :], wt[i][0:P, 0:1], None, ALU.mult),
                     reads=["ya%d" % i, "wt%d" % i], writes=["tmp"])
                S.op("dve", lambda e, i=i, P=P: e.scalar_tensor_tensor(tmp[0:P, :], ybt[i][0:P, :], wt[i][0:P, 1:2], tmp[0:P, :], ALU.mult, ALU.add),
                     reads=["yb%d" % i, "wt%d" % i, "tmp"], writes=["tmp"])
                S.op("pool", lambda e, P=P: e.tensor_tensor(tmp[0:P, :], tmp[0:P, :], g2b[0:P, :], ALU.mult),
                     reads=["tmp"] + BK4("g2b"), writes=["tmp"])
                S.op("pool", lambda e, xa=xa, P=P: e.tensor_tensor(xa, xa, tmp[0:P, :], ALU.add), reads=["tmp", kx], writes=[kx])
                for h in range(2):
                    S.dma("act", xo[base + r0:base + r0 + P, h * 2048:(h + 1) * 2048], xt[i][0:P, h * 2048:(h + 1) * 2048], reads=[kx])
            if do_norm:
                emit_modnorm(S, xa, P, gsb[0:P, :], shb[0:P, :], hb[i][0:P, :], tmp[0:P, :], junk[0:P, :], stt[i][0:P, :],
                             [kx], BK4("gsb"), BK4("shb"), ["hb%d" % i], ["tmp"], ["junk"], ["st%d" % i])
                S.dma("act", ho[base + r0:base + r0 + P, :], hb[i][0:P, :], reads=["hb%d" % i])
            r0 += P
            n += 1
    return cx.finish()


class Arena:
    def __init__(self, ap, n):
        self.ap, self.n, self.off = ap, n, 0

    def reset(self):
        self.off = 0

    def _shape(self, v, shape):
        if len(shape) == 1:
            return v
        if len(shape) == 2:
            return v.rearrange("p (a b) -> p a b", a=shape[0])
        return v.rearrange("p (a b c) -> p a b c", a=shape[0], b=shape[1])

    def bf(self, *shape):
        n = int(np.prod(shape))
        n2 = n + (n % 2)
        v = self.ap[:, self.off:self.off + n]
        self.off += n2
        assert self.off <= self.n, "arena overflow %d" % self.off
        return self._shape(v, shape)

    def f32(self, *shape):
        n = int(np.prod(shape)) * 2
        v = self.ap[:, self.off:self.off + n].bitcast(F32)
        self.off += n
        assert self.off <= self.n, "arena overflow %d" % self.off
        return self._shape(v, shape)


FM_KIND = ["nrA", "nrA", "nrA", "nB", "nB", "nB", "nB", "nrC", "nrC", "nrC", "nrC", "scale", "scale", "copy", "copy", "silu", "silu"]
FM_GAIN = [0, 0, 1, 2, 2, 3, 3, 4, 4, 5, 5, -1, -1, -1, -1, -1, -1]
PASSES = [(list(range(0, 7)), [0, 1, 2]), (list(range(7, 11)), [3, 4]), (list(range(11, 17)), [5, 6, 7, 8])]


def build_A(S_, with_ctx, lam_init, nBpat, bpat_of_block, b_rlo_of_block, phases=("P", "A", "B", "C", "D")):
    cx = Ctx()
    S = cx.S
    NT = S_ + CTX
    NLB = S_ // 512
    blocks = [(i * 512, 512, True) for i in range(NLB)] + [(S_, CTX, False)]
    NKT = NT // 128
    NLT = S_ // 128
    hT = cx.din("hT", [D, NT], BF16)
    wfm = cx.din("wfm", [D, 17 * 128], F32)
    wtm = cx.din("wtm", [D, 9 * 128], F32)
    gains = cx.din("gains", [128, 8], F32)
    ropeA = cx.din("ropeA", [2, 128, S_], F32)
    ropeC = cx.din("ropeC", [2, 128, S_], F32)
    rmat = cx.din("rmat", [4, 128, 128], F32)
    maskA = cx.din("maskA", [6, 128, 512], F32)
    biasB = cx.din("biasB", [2, nBpat, 8, 128, 512], F32)
    sinks = cx.din("sinks", [128, 2], F32)
    lamc = cx.din("lamc", [128, 4, 64], F32)
    dtab = cx.din("dtab", [6, 128, 128], F32)
    dcol = cx.din("dcol", [128, 2], F32)
    lgd = cx.din("lgd", [128, 4], F32)
    oT = cx.dout("oT", [8 * 128, NT], BF16)
    import os
    _dbg = bool(os.environ.get("KDBG"))
    QT = (cx.dout if _dbg else cx.dscr)("QT", [17, 128, NT], BF16)
    VT = (cx.dout if _dbg else cx.dscr)("VT", [9, NT, 128], BF16)
    ps = cx.psum()
    PK = lambda i: "ps%d" % i

    cm = cx.sb("cm", [128, 4, 128], BF16)
    for i in range(4):
        S.dma("pool", cm[:, i, :], rmat[i], writes=["cm"])
    RA, RC, ONES, BONES = cm[:, 0, :], cm[:, 1, :], cm[:, 2, :], cm[:, 3, :]
    gn = cx.sb("gn", [128, 8], F32)
    S.dma("sp", gn, gains, writes=["gn"])
    sm = cx.sb("sm", [128, 32], F32)
    S.dma("sp", sm[:, 0:2], sinks, writes=["sm_sink"])
    S.op("act", lambda e: e.activation(out=sm[:, 2:4], in_=sm[:, 0:2], func=AF.Exp), reads=["sm_sink"], writes=["sm_esink"])
    ARN = 96 * 1024
    art = cx.sb("arena", [128, ARN], BF16)
    ar = Arena(art, ARN)

    def rstd_from(ss_ps, N, dh, dst, kps, kdst):
        S.op("dve", lambda e: e.tensor_scalar(dst[:, 0:N], ss_ps[:, 0:N], 1.0 / dh, EPS, ALU.mult, ALU.add), reads=[kps], writes=[kdst])
        S.op("act", lambda e: e.activation(out=dst[:, 0:N], in_=dst[:, 0:N], func=AF.Sqrt), reads=[kdst], writes=[kdst])
        S.op("dve", lambda e: e.reciprocal(dst[:, 0:N], dst[:, 0:N]), reads=[kdst], writes=[kdst])

    if "P" in phases:
        ar.reset()
        hb = [ar.bf(32, 512) for _ in range(2)]
        wreg = ar.bf(32, 1280)
        sq = [ar.bf(512) for _ in range(2)]
        qn = [ar.bf(512) for _ in range(2)]
        ob = [ar.bf(512) for _ in range(4)]
        tb_ = [ar.bf(512) for _ in range(2)]
        rs = [ar.f32(512) for _ in range(2)]
        t1 = [ar.f32(512) for _ in range(2)]
        t2 = [ar.f32(512) for _ in range(2)]
        rp = [ar.f32(4, 512) for _ in range(2)]
        cnt = dict(hb=0, ep=0, ob=0, tm=0, mm=0)
        for pi, (fms, tms) in enumerate(PASSES):
            nf, nt_ = len(fms), len(tms)
            for wi, f in enumerate(fms):
                for h in range(8):
                    S.dma("pool", wreg[:, h * 4:(h + 1) * 4, wi * 128:(wi + 1) * 128],
                          wfm.rearrange("(c p) n -> p c n", p=128)[:, h * 4:(h + 1) * 4, f * 128:(f + 1) * 128], writes=["w%d" % wi])
            for wi, t in enumerate(tms):
                for h in range(8):
                    S.dma("pool", wreg[:, h * 4:(h + 1) * 4, (nf + wi) * 128:(nf + wi + 1) * 128],
                          wtm.rearrange("(c p) n -> p c n", p=128)[:, h * 4:(h + 1) * 4, t * 128:(t + 1) * 128], writes=["w%d" % (nf + wi)])
            for bi, (t0, N, lat) in enumerate(blocks):
                hbi = cnt["hb"] % 2
                cnt["hb"] += 1
                h_ = hb[hbi]
                kh = "hb%d" % hbi
                for h in range(8):
                    S.dma("sp", h_[:, h * 4:(h + 1) * 4, 0:N], hT.rearrange("(c p) n -> p c n", p=128)[:, h * 4:(h + 1) * 4, t0:t0 + N],
                          writes=[kh + "_%d" % h])
                khs = [kh + "_%d" % h for h in range(8)]
                need_rope = lat and any(FM_KIND[f].startswith("nr") for f in fms)
                rpi = bi % 2
                if need_rope:
                    if pi == 0:
                        S.dma("act", rp[rpi][:, 0, :], ropeA[0][:, t0:t0 + N], writes=["rp%d" % rpi])
                        S.dma("act", rp[rpi][:, 1, :], ropeA[1][:, t0:t0 + N], writes=["rp%d" % rpi])
                    else:
                        S.dma("act", rp[rpi][:, 2, :], ropeC[0][:, t0:t0 + N], writes=["rp%d" % rpi])
                        S.dma("act", rp[rpi][:, 3, :], ropeC[1][:, t0:t0 + N], writes=["rp%d" % rpi])
                for wi, f in enumerate(fms):
                    kind = FM_KIND[f]
                    m = cnt["mm"] % 2
                    cnt["mm"] += 1
                    pm = ps[m]
                    for k in range(32):
                        S.op("pe", lambda e, pm=pm, wi=wi, k=k, h_=h_, N=N: e.matmul(pm[:, 0:N], wreg[:, k, wi * 128:(wi + 1) * 128], h_[:, k, 0:N],
                                                                                 start=(k == 0), stop=(k == 31)),
                             reads=["w%d" % wi, khs[k // 4]], writes=[PK(m)])
                    oi = cnt["ob"] % 4
                    cnt["ob"] += 1
                    o_ = ob[oi]
                    ko = "ob%d" % oi
                    ei = cnt["ep"] % 2
                    cnt["ep"] += 1
                    if kind in ("scale", "copy", "silu"):
                        fn = AF.Silu if kind == "silu" else AF.Copy
                        sc_ = HD ** -0.5 if kind == "scale" else 1.0
                        S.op("act", lambda e, o_=o_, pm=pm, N=N, fn=fn, sc_=sc_: e.activation(out=o_[:, 0:N], in_=pm[:, 0:N], func=fn, scale=sc_),
                             reads=[PK(m)], writes=[ko])
                    else:
                        dh = 64 if kind == "nrC" else 128
                        onesm = BONES if kind == "nrC" else ONES
                        sq_, qn_, rs_, t1_, t2_ = sq[ei], qn[ei], rs[ei], t1[ei], t2[ei]
                        ke = "e%d" % ei
                        S.op("act", lambda e, sq_=sq_, pm=pm, N=N: e.activation(out=sq_[:, 0:N], in_=pm[:, 0:N], func=AF.Square),
                             reads=[PK(m)], writes=[ke + "sq"])
                        p2 = ps[2 + ei]
                        S.op("pe", lambda e, p2=p2, onesm=onesm, sq_=sq_, N=N: e.matmul(p2[:, 0:N], onesm, sq_[:, 0:N], start=True, stop=True),
                             reads=[ke + "sq", "cm"], writes=[PK(2 + ei)])
                        rstd_from(p2, N, dh, rs_, PK(2 + ei), ke + "rs")
                        gcol = gn[:, FM_GAIN[f]:FM_GAIN[f] + 1]
                        rope = lat and kind in ("nrA", "nrC")
                        dst = qn_ if rope else o_
                        kd = (ke + "qn") if rope else ko
                        S.op("dve", lambda e, dst=dst, pm=pm, gcol=gcol, rs_=rs_, N=N: e.scalar_tensor_tensor(dst[:, 0:N], pm[:, 0:N], gcol, rs_[:, 0:N], ALU.mult, ALU.mult),
                             reads=[PK(m), "gn", ke + "rs"], writes=[kd])
                        if rope:
                            Rm = RA if kind == "nrA" else RC
                            ci = 0 if kind == "nrA" else 2
                            p3 = ps[4 + ei]
                            S.op("pe", lambda e, p3=p3, Rm=Rm, qn_=qn_, N=N: e.matmul(p3[:, 0:N], Rm, qn_[:, 0:N], start=True, stop=True),
                                 reads=[ke + "qn", "cm"], writes=[PK(4 + ei)])
                            S.op("pool", lambda e, t1_=t1_, qn_=qn_, ci=ci, rpi=rpi, N=N: e.tensor_tensor(t1_[:, 0:N], qn_[:, 0:N], rp[rpi][:, ci, 0:N], ALU.mult),
                                 reads=[ke + "qn", "rp%d" % rpi], writes=[ke + "t1"])
                            S.op("dve", lambda e, t2_=t2_, p3=p3, ci=ci, rpi=rpi, N=N: e.tensor_tensor(t2_[:, 0:N], p3[:, 0:N], rp[rpi][:, ci + 1, 0:N], ALU.mult),
                                 reads=[PK(4 + ei), "rp%d" % rpi], writes=[ke + "t2"])
                            S.op("pool", lambda e, o_=o_, t1_=t1_, t2_=t2_, N=N: e.tensor_tensor(o_[:, 0:N], t1_[:, 0:N], t2_[:, 0:N], ALU.add),
                                 reads=[ke + "t1", ke + "t2"], writes=[ko])
                    S.dma("sp", QT[f][:, t0:t0 + N], o_[:, 0:N], reads=[ko], writes=["QT%d_%d" % (f, bi)])
                ncol = nt_ * 128
                for sub in range(N // 128):
                    m = 6 + cnt["tm"] % 2
                    ti = cnt["tm"] % 2
                    cnt["tm"] += 1
                    pm = ps[m]
                    for k in range(32):
                        S.op("pe", lambda e, pm=pm, k=k, h_=h_, sub=sub, ncol=ncol, nf=nf: e.matmul(pm[:, 0:ncol], h_[:, k, sub * 128:(sub + 1) * 128],
                                                                                                   wreg[:, k, nf * 128:nf * 128 + ncol], start=(k == 0), stop=(k == 31)),
                             reads=["w%d" % (nf + wi_) for wi_ in range(nt_)] + [khs[k // 4]], writes=[PK(m)])
                    tb1 = tb_[ti]
                    S.op("act", lambda e, tb1=tb1, pm=pm, ncol=ncol: e.activation(out=tb1[:, 0:ncol], in_=pm[:, 0:ncol], func=AF.Copy),
                         reads=[PK(m)], writes=["tb%d" % ti])
                    for wi_, t in enumerate(tms):
                        S.dma("act", VT[t][t0 + sub * 128:t0 + (sub + 1) * 128, :], tb1[:, wi_ * 128:(wi_ + 1) * 128], reads=["tb%d" % ti],
                              writes=["VT%d_%d" % (t, bi)])
        S.barrier()
    QK = lambda f: ["QT%d_%d" % (f, bi) for bi in range(len(blocks))]
    VK = lambda t: ["VT%d_%d" % (t, bi) for bi in range(len(blocks))]

    ar.reset()
    kT = ar.bf(NT)
    qT = ar.bf(NT)
    vv = ar.bf(NKT, 128)
    Pt = [ar.bf(512) for _ in range(3)]
    tf = [ar.f32(512) for _ in range(2)]
    rl = [ar.f32(512) for _ in range(2)]
    on = [ar.f32(512) for _ in range(3)]
    osb = [ar.bf(512) for _ in range(2)]
    state = dict(p=0, s=0, o=0)

    def load_fm(dst, f, key, q="sp"):
        for c in range(0, NT, 2048):
            n = min(2048, NT - c)
            S.dma(q, dst[:, c:c + n], QT[f][:, c:c + n], reads=QK(f), writes=[key])

    def load_v(t, key="vv"):
        for c in range(0, NKT, 4):
            n = min(4, NKT - c)
            S.dma("act", vv[:, c:c + n, :], VT[t][c * 128:(c + n) * 128, :].rearrange("(c p) d -> p c d", p=128), reads=VK(t), writes=[key])

    def attn_block(q_ap, N, keys, scale, o_idx, l_idx, kpart=None, qkey="qT", kkey="kT"):
        nk = len(keys)
        for i, (kt, bias, bkey) in enumerate(keys):
            si = state["s"] % 2
            state["s"] += 1
            ps_s = ps[si]
            lo, hi = kpart if kpart else (0, 128)
            S.op("pe", lambda e, ps_s=ps_s, kt=kt, lo=lo, hi=hi, N=N, q_ap=q_ap: e.matmul(ps_s[:, 0:N], kT[lo:hi, kt * 128:(kt + 1) * 128], q_ap[lo:hi, 0:N],
                                                                                     start=True, stop=True),
                 reads=[kkey, qkey], writes=[PK(si)])
            pi_ = state["p"] % 3
            state["p"] += 1
            P_ = Pt[pi_]
            kp = "P%d" % pi_
            if bias is not None:
                ti = state["p"] % 2
                tf_ = tf[ti]
                S.op("dve", lambda e, tf_=tf_, ps_s=ps_s, bias=bias, N=N: e.scalar_tensor_tensor(tf_[:, 0:N], ps_s[:, 0:N], scale, bias[:, 0:N], ALU.mult, ALU.add),
                     reads=[PK(si), bkey], writes=["tf%d" % ti])
                S.op("act", lambda e, P_=P_, tf_=tf_, N=N: e.activation(out=P_[:, 0:N], in_=tf_[:, 0:N], func=AF.Exp), reads=["tf%d" % ti], writes=[kp])
            else:
                S.op("act", lambda e, P_=P_, ps_s=ps_s, N=N: e.activation(out=P_[:, 0:N], in_=ps_s[:, 0:N], func=AF.Exp, scale=scale), reads=[PK(si)], writes=[kp])
            S.op("pe", lambda e, kt=kt, P_=P_, N=N, i=i: e.matmul(ps[o_idx][:, 0:N], vv[:, kt, :], P_[:, 0:N], start=(i == 0), stop=(i == nk - 1)),
                 reads=[kp, "vv"], writes=[PK(o_idx)])
            S.op("pe", lambda e, P_=P_, N=N, i=i: e.matmul(ps[l_idx][:, 0:N], ONES, P_[:, 0:N], start=(i == 0), stop=(i == nk - 1)),
                 reads=[kp, "cm"], writes=[PK(l_idx)])

    def finish_softmax(N, o_idx, l_idx, sink_col, out_ap, kout, f32_out=False):
        ri = state["o"] % 2
        state["o"] += 1
        rl_ = rl[ri]
        kr = "rl%d" % ri
        if sink_col is not None:
            S.op("dve", lambda e: e.tensor_scalar(rl_[:, 0:N], ps[l_idx][:, 0:N], sink_col, None, ALU.add), reads=[PK(l_idx), "sm_esink"], writes=[kr])
            S.op("dve", lambda e: e.reciprocal(rl_[:, 0:N], rl_[:, 0:N]), reads=[kr], writes=[kr])
        else:
            S.op("dve", lambda e: e.reciprocal(rl_[:, 0:N], ps[l_idx][:, 0:N]), reads=[PK(l_idx)], writes=[kr])
        S.op("dve", lambda e: e.tensor_tensor(out_ap[:, 0:N], ps[o_idx][:, 0:N], rl_[:, 0:N], ALU.mult), reads=[PK(o_idx), kr], writes=[kout])

    def store_o(slot, t0, N, src, ksrc):
        S.dma("sp", oT[slot * 128:(slot + 1) * 128, t0:t0 + N], src[:, 0:N], reads=[ksrc], writes=["oT"])

    ctx_keys = [(NLT + i, None, None) for i in range(CTX // 128)]
    qblocks = [(i * 512, 512) for i in range(NLB)]

    if "A" in phases:
        mA = ar.f32(6, 512)
        for i in range(6):
            S.dma("act", mA[:, i, :], maskA[i], writes=["mA"])
        load_fm(kT, 2, "kT")
        load_v(0)
        for hq in range(2):
            load_fm(qT, hq, "qT")
            sc_ = HD ** -0.5
            for (q0, N) in qblocks:
                k_lo, k_hi = max(0, q0 - 128), min(S_, q0 + 640)
                keys = [(k0 // 128, mA[:, (k0 - q0) // 128 + 1, :], "mA") for k0 in range(k_lo, k_hi, 128)] + ctx_keys
                oi, li = 2 + (state["o"] % 2), 4 + (state["o"] % 2)
                attn_block(qT[:, q0:q0 + N], N, keys, sc_, oi, li)
                ob_ = osb[state["o"] % 2]
                ko = "osb%d" % (state["o"] % 2)
                finish_softmax(N, oi, li, sm[:, 2 + hq:3 + hq], ob_, ko)
                store_o(hq, q0, N, ob_, ko)
            if with_ctx:
                N = CTX
                oi, li = 2 + (state["o"] % 2), 4 + (state["o"] % 2)
                attn_block(qT[:, S_:S_ + N], N, ctx_keys, sc_, oi, li)
                ob_ = osb[state["o"] % 2]
                ko = "osb%d" % (state["o"] % 2)
                finish_softmax(N, oi, li, sm[:, 2 + hq:3 + hq], ob_, ko)
                store_o(hq, S_, N, ob_, ko)
        S.barrier()
    base_off = ar.off

    if "B" in phases:
        ar.off = base_off
        bB = [ar.f32(8, 512) for _ in range(2)]
        nb = 0
        for hq in range(2):
            load_fm(kT, 5 + hq, "kT")
            load_fm(qT, 3 + hq, "qT")
            load_v(1 + hq)
            sc_ = HD ** -0.5
            cur_pat = None
            for blk, (q0, N) in enumerate(qblocks):
                pat = bpat_of_block[blk]
                if pat != cur_pat:
                    bi_ = nb % 2
                    nb += 1
                    for i in range(8):
                        S.dma("act", bB[bi_][:, i, :], biasB[hq, pat, i], writes=["bB%d" % bi_])
                    cur_pat = pat
                    cur_b = bi_
                r_lo = b_rlo_of_block[blk]
                keys = [(r_lo // 2 + i, bB[cur_b][:, i, :], "bB%d" % cur_b) for i in range(8)] + ctx_keys
                oi, li = 2 + (state["o"] % 2), 4 + (state["o"] % 2)
                attn_block(qT[:, q0:q0 + N], N, keys, sc_, oi, li)
                ob_ = osb[state["o"] % 2]
                ko = "osb%d" % (state["o"] % 2)
                finish_softmax(N, oi, li, None, ob_, ko)
                store_o(2 + hq, q0, N, ob_, ko)
            if with_ctx:
                N = CTX
                oi, li = 2 + (state["o"] % 2), 4 + (state["o"] % 2)
                attn_block(qT[:, S_:S_ + N], N, ctx_keys, sc_, oi, li)
                ob_ = osb[state["o"] % 2]
                ko = "osb%d" % (state["o"] % 2)
                finish_softmax(N, oi, li, None, ob_, ko)
                store_o(2 + hq, S_, N, ob_, ko)
        S.barrier()

    if "C" in phases:
        ar.off = base_off
        lt = ar.f32(4, 64)
        S.dma("sp", lt, lamc, writes=["lt"])
        S.op("dve", lambda e: e.tensor_tensor(lt[:, 0, :], lt[:, 0, :], lt[:, 1, :], ALU.mult), reads=["lt"], writes=["lt"])
        S.op("dve", lambda e: e.tensor_tensor(lt[:, 2, :], lt[:, 2, :], lt[:, 3, :], ALU.mult), reads=["lt"], writes=["lt"])
        S.op("dve", lambda e: e.tensor_reduce(out=sm[:, 8:9], in_=lt[:, 0, :], axis=AX.X, op=ALU.add), reads=["lt"], writes=["sm_lam"])
        S.op("dve", lambda e: e.tensor_reduce(out=sm[:, 9:10], in_=lt[:, 2, :], axis=AX.X, op=ALU.add), reads=["lt"], writes=["sm_lam"])
        S.op("act", lambda e: e.activation(out=sm[:, 10:12], in_=sm[:, 8:10], func=AF.Exp), reads=["sm_lam"], writes=["sm_lam"])
        S.op("dve", lambda e: e.tensor_tensor(sm[:, 12:13], sm[:, 11:12], sm[:, 10:11], ALU.subtract), reads=["sm_lam"], writes=["sm_lam"])
        S.op("dve", lambda e: e.tensor_scalar(sm[:, 12:13], sm[:, 12:13], -lam_init, None, ALU.add), reads=["sm_lam"], writes=["sm_lam"])
        S.op("dve", lambda e: e.tensor_scalar(sm[:, 13:14], gn[:, 6:7], 1.0 - lam_init, None, ALU.mult), reads=["gn"], writes=["sm_sub"])
        sqc = ar.bf(512)
        sc_ = 64 ** -0.5
        all_keys = [(i, None, None) for i in range(NKT)]

        def c_block(q0, N, keys, slot):
            attn_block(qT[:, q0:q0 + N], N, keys, sc_, 2, 4, kpart=(0, 64))
            attn_block(qT[:, q0:q0 + N], N, keys, sc_, 3, 5, kpart=(64, 128))
            finish_softmax(N, 2, 4, None, on[0], "on0")
            finish_softmax(N, 3, 5, None, on[1], "on1")
            S.op("dve", lambda e: e.scalar_tensor_tensor(on[2][:, 0:N], on[1][:, 0:N], sm[:, 12:13], on[0][:, 0:N], ALU.mult, ALU.add),
                 reads=["on0", "on1", "sm_lam"], writes=["on2"])
            S.op("act", lambda e: e.activation(out=sqc[:, 0:N], in_=on[2][:, 0:N], func=AF.Square), reads=["on2"], writes=["sqc"])
            S.op("pe", lambda e: e.matmul(ps[6][:, 0:N], ONES, sqc[:, 0:N], start=True, stop=True), reads=["sqc", "cm"], writes=[PK(6)])
            rstd_from(ps[6], N, 128, tf[0], PK(6), "tf0")
            ob_ = osb[state["o"] % 2]
            ko = "osb%d" % (state["o"] % 2)
            S.op("dve", lambda e: e.scalar_tensor_tensor(ob_[:, 0:N], on[2][:, 0:N], sm[:, 13:14], tf[0][:, 0:N], ALU.mult, ALU.mult),
                 reads=["on2", "sm_sub", "tf0"], writes=[ko])
            store_o(slot, q0, N, ob_, ko)

        for hq in range(2):
            load_fm(kT, 9 + hq, "kT")
            load_fm(qT, 7 + hq, "qT")
            load_v(3 + hq)
            for (q0, N) in qblocks:
                c_block(q0, N, all_keys, 4 + hq)
            if with_ctx:
                c_block(S_, CTX, ctx_keys, 4 + hq)
        S.barrier()

    if "D" in phases:
        ar.off = base_off
        gT = ar.bf(NT)
        ktm = ar.bf(NKT, 128)
        osum = ar.f32(NT)
        osum2 = ar.f32(NT)
        dtb = ar.f32(6, 128)
        for t_ in range(6):
            S.dma("sp", dtb[:, t_, :], dtab[t_], writes=["dtb"])
        dcl = ar.f32(2)
        S.dma("sp", dcl, dcol, writes=["dcl"])
        lg = ar.f32(4)
        S.dma("sp", lg, lgd, writes=["lg"])
        intra = [ar.bf(128) for _ in range(2)]
        qdec = [ar.f32(128) for _ in range(2)]
        kdec = ar.f32(4)
        tfd = ar.f32(128)
        PTd = [ar.bf(128) for _ in range(2)]
        qd = [ar.bf(128) for _ in range(2)]
        kdk = [ar.bf(128) for _ in range(2)]
        Sf = ar.f32(128)
        Sb = [ar.bf(128) for _ in range(2)]
        sqd = ar.bf(512)
        ctx_chunks_f = [NLT + i for i in range(CTX // 128)]
        lat_chunks_f = list(range(NLT))
        for hq in range(2):
            load_fm(kT, 13 + hq, "kT")
            load_fm(qT, 11 + hq, "qT")
            load_fm(gT, 15 + hq, "gT")
            load_v(5 + hq)
            for c in range(0, NKT, 4):
                n = min(4, NKT - c)
                S.dma("act", ktm[:, c:c + n, :], VT[7 + hq][c * 128:(c + n) * 128, :].rearrange("(c p) d -> p c d", p=128), reads=VK(7 + hq), writes=["ktm"])
            for dr in range(2):
                lgc = lg[:, dr * 2 + hq:dr * 2 + hq + 1]
                S.op("act", lambda e, dr=dr, lgc=lgc: e.activation(out=tfd, in_=dtb[:, 2 * dr, :], func=AF.Exp, scale=lgc), reads=["dtb", "lg"], writes=["tfd"])
                S.op("dve", lambda e, dr=dr: e.tensor_tensor(intra[dr], tfd, dtb[:, 2 * dr + 1, :], ALU.mult), reads=["tfd", "dtb"], writes=["intra%d" % dr])
                S.op("act", lambda e, dr=dr, lgc=lgc: e.activation(out=qdec[dr], in_=dtb[:, 4 + dr, :], func=AF.Exp, scale=lgc), reads=["dtb", "lg"], writes=["qdec%d" % dr])
                S.op("act", lambda e, dr=dr, lgc=lgc: e.activation(out=kdec[:, dr:dr + 1], in_=dcl[:, dr:dr + 1], func=AF.Exp, scale=lgc), reads=["dcl", "lg"], writes=["kdec"])
                S.op("act", lambda e, dr=dr, lgc=lgc: e.activation(out=kdec[:, 2 + dr:3 + dr], in_=lgc, func=AF.Exp, scale=128.0), reads=["lg"], writes=["kdec"])
                order = (ctx_chunks_f + lat_chunks_f) if dr == 0 else (ctx_chunks_f[::-1] + lat_chunks_f[::-1])
                S.op("pool", lambda e: e.memset(Sf, 0.0), writes=["Sf"])
                S.op("pool", lambda e: e.memset(Sb[0], 0.0), writes=["Sb0"])
                sbi = 0
                for ci, ch in enumerate(order):
                    t0 = ch * 128
                    is_ctx = ch >= NLT
                    want_out = (not is_ctx) or with_ctx
                    i2 = ci % 2
                    if want_out:
                        S.op("pe", lambda e, t0=t0: e.matmul(ps[0][:, 0:128], kT[:, t0:t0 + 128], qT[:, t0:t0 + 128], start=True, stop=True),
                             reads=["kT", "qT"], writes=[PK(0)])
                        S.op("dve", lambda e, i2=i2, dr=dr: e.tensor_tensor(PTd[i2], ps[0][:, 0:128], intra[dr], ALU.mult),
                             reads=[PK(0), "intra%d" % dr], writes=["PTd%d" % i2])
                        S.op("dve", lambda e, i2=i2, dr=dr, t0=t0: e.tensor_tensor(qd[i2], qT[:, t0:t0 + 128], qdec[dr], ALU.mult),
                             reads=["qT", "qdec%d" % dr], writes=["qd%d" % i2])
                        S.op("pe", lambda e, ch=ch, i2=i2: e.matmul(ps[2][:, 0:128], vv[:, ch, :], PTd[i2], start=True, stop=False),
                             reads=["vv", "PTd%d" % i2], writes=[PK(2)])
                        S.op("pe", lambda e, i2=i2, sbi=sbi: e.matmul(ps[2][:, 0:128], Sb[sbi], qd[i2], start=False, stop=True),
                             reads=["Sb%d" % sbi, "qd%d" % i2], writes=[PK(2)])
                        if dr == 0:
                            S.op("act", lambda e, t0=t0: e.activation(out=osum[:, t0:t0 + 128], in_=ps[2][:, 0:128], func=AF.Copy), reads=[PK(2)], writes=["osum"])
                        else:
                            S.op("act", lambda e, t0=t0: e.activation(out=osum2[:, t0:t0 + 128], in_=ps[2][:, 0:128], func=AF.Copy), reads=[PK(2)], writes=["osum2"])
                    if ci < len(order) - 1:
                        S.op("dve", lambda e, i2=i2, ch=ch, dr=dr: e.tensor_scalar(kdk[i2], ktm[:, ch, :], kdec[:, dr:dr + 1], None, ALU.mult),
                             reads=["ktm", "kdec"], writes=["kdk%d" % i2])
                        S.op("pe", lambda e, i2=i2, ch=ch: e.matmul(ps[4][:, 0:128], kdk[i2], vv[:, ch, :], start=True, stop=True),
                             reads=["kdk%d" % i2, "vv"], writes=[PK(4)])
                        S.op("dve", lambda e, dr=dr: e.scalar_tensor_tensor(Sf, Sf, kdec[:, 2 + dr:3 + dr], ps[4][:, 0:128], ALU.mult, ALU.add),
                             reads=["Sf", "kdec", PK(4)], writes=["Sf"])
                        sbi = 1 - sbi
                        S.op("act", lambda e, sbi=sbi: e.activation(out=Sb[sbi], in_=Sf, func=AF.Copy), reads=["Sf"], writes=["Sb%d" % sbi])
            if _dbg and hq == 0:
                dbg = cx.dout("dbg", [2, 128, NT], F32)
                S.dma("sp", dbg[0], osum, reads=["osum"], writes=["dbg"])
                S.dma("sp", dbg[1], osum2, reads=["osum2"], writes=["dbg"])
            fin_blocks = qblocks + ([(S_, CTX)] if with_ctx else [])
            for (q0, N) in fin_blocks:
                S.op("dve", lambda e, q0=q0, N=N: e.tensor_tensor(osum[:, q0:q0 + N], osum[:, q0:q0 + N], osum2[:, q0:q0 + N], ALU.add),
                     reads=["osum", "osum2"], writes=["osum"])
                S.op("act", lambda e, q0=q0, N=N: e.activation(out=sqd[:, 0:N], in_=osum[:, q0:q0 + N], func=AF.Square), reads=["osum"], writes=["sqd"])
                S.op("pe", lambda e, N=N: e.matmul(ps[6][:, 0:N], ONES, sqd[:, 0:N], start=True, stop=True), reads=["sqd", "cm"], writes=[PK(6)])
                rstd_from(ps[6], N, 128, tf[0], PK(6), "tf0")
                S.op("dve", lambda e, q0=q0, N=N: e.tensor_tensor(tf[1][:, 0:N], osum[:, q0:q0 + N], tf[0][:, 0:N], ALU.mult), reads=["osum", "tf0"], writes=["tf1"])
                ob_ = osb[state["o"] % 2]
                ko = "osb%d" % (state["o"] % 2)
                state["o"] += 1
                S.op("pool", lambda e, ob_=ob_, q0=q0, N=N: e.tensor_tensor(ob_[:, 0:N], tf[1][:, 0:N], gT[:, q0:q0 + N], ALU.mult), reads=["tf1", "gT"], writes=[ko])
                store_o(6 + hq, q0, N, ob_, ko)
    if not with_ctx:
        zt = cx.sb("zt", [128, CTX], BF16)
        S.op("pool", lambda e: e.memset(zt, 0.0), writes=["zt"])
        for sl in range(8):
            S.dma("sp", oT[sl * 128:(sl + 1) * 128, S_:S_ + CTX], zt, reads=["zt"], writes=["oT"])
    return cx.finish()


def rope_tables_fm(L, dim, reps):
    t = np.arange(L)
    row = (t // GRID_W).astype(np.float32)
    col = (t % GRID_W).astype(np.float32)
    quarter = dim // 4
    inv = (np.float32(10000.0) ** (-np.arange(quarter, dtype=np.float32) / np.float32(quarter))).astype(np.float32)
    a_r = row[:, None] * inv[None, :]
    a_c = col[:, None] * inv[None, :]
    ang = np.concatenate([a_r, a_r, a_c, a_c], axis=-1)
    cs = np.stack([np.cos(ang).T, np.sin(ang).T]).astype(np.float32)
    return np.ascontiguousarray(np.tile(cs, (1, reps, 1)))


def rot_lhsT(dim, reps):
    R = np.zeros((128, 128), np.float32)
    q = dim // 4
    for r in range(reps):
        o = r * dim
        for m in range(dim):
            blk = m // q
            if blk % 2 == 0:
                R[o + m + q, o + m] = -1.0
            else:
                R[o + m - q, o + m] = 1.0
    return R


def const_rmat():
    ones = np.ones((128, 128), np.float32)
    bones = np.zeros((128, 128), np.float32)
    bones[:64, :64] = 1.0
    bones[64:, 64:] = 1.0
    return np.stack([rot_lhsT(128, 1), rot_lhsT(64, 2), ones, bones])


def const_maskA():
    k = np.arange(128)[:, None]
    q = np.arange(512)[None, :]
    out = np.zeros((6, 128, 512), np.float32)
    for i in range(6):
        rel = (i - 1) * 128 + k - q
        out[i] = np.where(np.abs(rel) <= 128, 0.0, NEG)
    return out


def b_bias_block(rpb_h, r0, r_lo, R):
    qr = r0 + np.arange(8)
    rs = np.clip(qr - 4, 0, R - 8)
    kr = r_lo + np.arange(16)
    cidx = np.arange(64)
    cs = np.clip(cidx - 8, 0, 64 - 16)
    colmask = (cidx[None, :] >= cs[:, None]) & (cidx[None, :] < cs[:, None] + 16)
    rel_c = np.clip(cidx[None, :] - cidx[:, None] + 15, 0, 30)
    out = np.full((16, 64, 8, 64), NEG, np.float32)
    for qi in range(8):
        for ki in range(16):
            dr = kr[ki] - rs[qi]
            if 0 <= dr < 8:
                rel_r = kr[ki] - qr[qi] + 7
                vals = rpb_h[rel_r][rel_c]
                out[ki, :, qi, :] = np.where(colmask, vals, NEG).T
    return out.reshape(8, 128, 512)


def b_patterns(rpb2, S_):
    R = S_ // GRID_W
    nblk = S_ // 512
    pats, pat_of, rlo_of, seen = [], [], [], {}
    for blk in range(nblk):
        r0 = blk * 8
        r_lo = int(np.clip(r0 - 4, 0, R - 16))
        key = (r0 - r_lo, r0 == 0 or r0 < 4, r0 + 8 + 3 > R - 1 + 0 and (R - 8) - (r0 + 7 - 4) < 0 or r0 + 12 > R)
        key = (r0 - r_lo, tuple(np.clip(r0 + np.arange(8) - 4, 0, R - 8) - r0))
        if key not in seen:
            seen[key] = len(pats)
            pats.append(np.stack([b_bias_block(rpb2[h], r0, r_lo, R) for h in range(2)]))
        pat_of.append(seen[key])
        rlo_of.append(r_lo)
    biasB = np.ascontiguousarray(np.stack(pats, axis=1))
    return biasB, pat_of, rlo_of


def const_dtab():
    j = np.arange(128)[:, None].astype(np.float32)
    i = np.arange(128)[None, :].astype(np.float32)
    relF = np.maximum(i - j, 0.0)
    maskF = (i >= j).astype(np.float32)
    relB = np.maximum(j - i, 0.0)
    maskB = (j >= i).astype(np.float32)
    posF = np.broadcast_to(i + 1.0, (128, 128))
    posB = np.broadcast_to(128.0 - i, (128, 128))
    dtab = np.stack([relF, maskF, relB, maskB, posF, posB]).astype(np.float32)
    jj = np.arange(128).astype(np.float32)
    dcol = np.stack([127.0 - jj, jj], axis=1).astype(np.float32)
    return np.ascontiguousarray(dtab), np.ascontiguousarray(dcol)


IN_OFF = dict(Aq=0, Ak=1024, Av=1280, Bq=1536, Bk=2560, Bv=3584, Cq=4608, Ck=5632, Cv=6656, Dq=7680, Dk=8704, Dv=9728, Dg=10752)


def a_weight_cols(j):
    h0, h1, kv = 2 * j, 2 * j + 1, j // 2
    c = lambda g, h: list(range(IN_OFF[g] + h * 128, IN_OFF[g] + (h + 1) * 128))
    fm = (c("Aq", h0) + c("Aq", h1) + c("Ak", kv) + c("Bq", h0) + c("Bq", h1) + c("Bk", h0) + c("Bk", h1) + c("Cq", h0) + c("Cq", h1)
          + c("Ck", h0) + c("Ck", h1) + c("Dq", h0) + c("Dq", h1) + c("Dk", h0) + c("Dk", h1) + c("Dg", h0) + c("Dg", h1))
    tm = c("Av", kv) + c("Bv", h0) + c("Bv", h1) + c("Cv", h0) + c("Cv", h1) + c("Dv", h0) + c("Dv", h1) + c("Dk", h0) + c("Dk", h1)
    return fm, tm


def bc128(v):
    return np.ascontiguousarray(np.broadcast_to(np.asarray(v, np.float32)[None], (128,) + np.asarray(v).shape))


def prep_A(p, l, S_, hT_b):
    import math
    lam_init = 0.8 - 0.6 * math.exp(-0.3 * l)
    ropeA = rope_tables_fm(S_, 128, 1)
    ropeC = rope_tables_fm(S_, 64, 2)
    rmat = const_rmat()
    maskA = const_maskA()
    dtab, dcol = const_dtab()
    ims = []
    bargs = None
    for i in range(NCORES):
        b, j = i // 4, i % 4
        h0, h1 = 2 * j, 2 * j + 1
        fm, tm = a_weight_cols(j)
        w_in = p["w_in"][l]
        gains = np.zeros((128, 8), np.float32)
        gains[:, 0] = p["qk_norm_a"][l][0]
        gains[:, 1] = p["qk_norm_a"][l][1]
        gains[:, 2] = p["qk_norm_b"][l][0]
        gains[:, 3] = p["qk_norm_b"][l][1]
        gains[:, 4] = np.tile(p["qk_norm_c"][l][0], 2)
        gains[:, 5] = np.tile(p["qk_norm_c"][l][1], 2)
        gains[:, 6] = p["subln_c"][l]
        biasB, pat_of, rlo_of = b_patterns(p["rpb_b"][l][[h0, h1]], S_)
        bargs = (biasB.shape[1], pat_of, rlo_of)
        rld = p["ret_log_decay"][l]
        ims.append(dict(hT=hT_b[b], wfm=np.ascontiguousarray(w_in[:, fm]), wtm=np.ascontiguousarray(w_in[:, tm]), gains=gains,
                        ropeA=ropeA, ropeC=ropeC, rmat=rmat, maskA=maskA, biasB=biasB,
                        sinks=bc128(p["sink_a"][l][[h0, h1]]), lamc=bc128(p["lambda_c"][l]), dtab=dtab, dcol=dcol,
                        lgd=bc128(np.array([rld[0, h0], rld[0, h1], rld[1, h0], rld[1, h1]], np.float32))))
    return lam_init, bargs, ims


def build_B(TOKL, TOKC):
    cx = Ctx()
    S = cx.S
    TOK = TOKL + TOKC
    oTo = cx.din("oTo", [D, TOK], BF16)
    x = cx.din("x", [TOK, D], F32)
    wo = cx.din("wo", [D, D], F32)
    g1 = cx.din("g1", [2, D], F32)
    nm = cx.din("nm", [D], F32)
    sc = cx.din("sc", [2, D], F32)
    sh = cx.din("sh", [2, D], F32)
    rt = cx.din("rt", [D, 36], F32)
    cst = cx.din("cst", [128, 160], F32)
    xmid = cx.dout("xmid", [TOK, D], F32)
    h2o = cx.dout("h2", [TOK, D], BF16)
    route = cx.dout("route", [TOK, 4], F32)
    ps = cx.psum()
    PK = lambda i: "ps%d" % i
    GT = 256
    oTg = cx.sb("oTg", [128, 32, GT], BF16)
    wsl = [cx.sb("wsl%d" % i, [128, 32, 256], BF16) for i in range(2)]
    xm = [cx.sb("xm%d" % i, [128, D], F32) for i in range(2)]
    xs = [cx.sb("xs%d" % i, [128, 256], F32) for i in range(2)]
    t2 = [cx.sb("t2%d" % i, [128, 256], F32) for i in range(2)]
    g1b = cx.sb("g1b", [128, D], F32)
    gsb = cx.sb("gsb", [128, D], F32)
    shb = cx.sb("shb", [128, D], F32)
    tmp = cx.sb("tmp", [128, D], F32)
    hbf = cx.sb("hbf", [128, D], BF16)
    h2T = cx.sb("h2T", [128, 32, 128], F32)
    rtt = cx.sb("rtt", [128, 32, 36], F32)
    cs = cx.sb("cs", [128, 160], F32)
    stt = cx.sb("stt", [128, 4], F32)
    sml = cx.sb("sml", [128, 128], F32)
    S.dma("sp", cs, cst, writes=["cs"])
    S.dma("sp", rtt, rt.rearrange("(c p) n -> p c n", p=128), writes=["rtt"])
    ident = cs[:, 0:128]
    iota = cs[:, 128:160]
    wov = wo.rearrange("(c p) n -> p c n", p=128)
    nW = 0
    groups = [(g * GT, GT, 0) for g in range(TOKL // GT)] + ([(TOKL, TOKC, 1)] if TOKC else [])
    cur_seg = None

    def route_tile(P, r0):
        KS = ["sml"]
        dv = lambda fn_, r=KS, w=KS: S.op("dve", fn_, reads=r, writes=w)
        c = [sml[0:P, 108 + i:109 + i] for i in range(8)]
        lg_, gl, el = sml[0:P, 0:36], sml[0:P, 0:4], sml[0:P, 4:36]
        gsel, pen, elm, oh, prod = sml[0:P, 36:40], sml[0:P, 40:44], sml[0:P, 44:76], sml[0:P, 76:108], sml[0:P, 0:32]
        S.op("dve", lambda e: e.tensor_copy(lg_, ps[6][0:P, 0:36]), reads=[PK(6)], writes=KS)
        dv(lambda e: e.tensor_reduce(out=c[0], in_=gl, axis=AX.X, op=ALU.max))
        dv(lambda e: e.tensor_scalar(gsel, gl, c[0], None, ALU.is_ge))
        dv(lambda e: e.tensor_scalar(c[1], c[0], -1.0, None, ALU.mult))
        S.op("act", lambda e: e.activation(out=pen, in_=gl, func=AF.Exp, bias=c[1], accum_out=c[2]), reads=KS, writes=KS)
        dv(lambda e: e.reciprocal(c[3], c[2]))
        dv(lambda e: e.tensor_scalar(pen, gsel, 1e9, -1e9, ALU.mult, ALU.add))
        for g in range(4):
            dv(lambda e, g=g: e.tensor_scalar(elm[:, g * 8:(g + 1) * 8], el[:, g * 8:(g + 1) * 8], pen[:, g:g + 1], None, ALU.add))
        dv(lambda e: e.tensor_reduce(out=c[4], in_=elm, axis=AX.X, op=ALU.max))
        dv(lambda e: e.tensor_scalar(oh, elm, c[4], None, ALU.is_ge))
        dv(lambda e: e.tensor_tensor(prod, oh, iota[0:P, :], ALU.mult))
        dv(lambda e: e.tensor_reduce(out=stt[0:P, 0:1], in_=prod, axis=AX.X, op=ALU.add), w=["stt", "sml"])
        dv(lambda e: e.scalar_tensor_tensor(elm, oh, -1e9, elm, ALU.mult, ALU.add))
        dv(lambda e: e.tensor_reduce(out=c[5], in_=elm, axis=AX.X, op=ALU.max))
        dv(lambda e: e.tensor_scalar(oh, elm, c[5], None, ALU.is_ge))
        dv(lambda e: e.tensor_tensor(prod, oh, iota[0:P, :], ALU.mult))
        dv(lambda e: e.tensor_reduce(out=stt[0:P, 1:2], in_=prod, axis=AX.X, op=ALU.add), w=["stt", "sml"])
        dv(lambda e: e.tensor_tensor(c[6], c[4], c[5], ALU.subtract))
        S.op("act", lambda e: e.activation(out=c[7], in_=c[6], func=AF.Sigmoid), reads=KS, writes=KS)
        dv(lambda e: e.tensor_tensor(stt[0:P, 2:3], c[7], c[3], ALU.mult), w=["stt", "sml"])
        dv(lambda e: e.tensor_tensor(stt[0:P, 3:4], c[3], stt[0:P, 2:3], ALU.subtract), r=["stt", "sml"], w=["stt"])
        S.dma("sp", route[r0:r0 + P, :], stt[0:P, :], reads=["stt"])

    for (g0, gn, seg) in groups:
        if seg != cur_seg:
            cur_seg = seg
            load_bcast(S, "act", g1b, g1[seg], "g1b")
            load_bcast(S, "act", gsb, sc[seg], "gsb")
            load_bcast(S, "act", shb, sh[seg], "shb")
            load_bcast(S, "act", tmp, nm, "tmpnm", also=["tmp"])
            S.op("dve", lambda e: e.scalar_tensor_tensor(gsb, gsb, 1.0, tmp, ALU.add, ALU.mult), reads=BK4("gsb") + BK4("tmpnm") + ["tmp"], writes=BK4("gsb") + ["tmp"])
        for h in range(8):
            S.dma("sp", oTg[:, h * 4:(h + 1) * 4, 0:gn], oTo.rearrange("(c p) t -> p c t", p=128)[:, h * 4:(h + 1) * 4, g0:g0 + gn], writes=["oTg_%d" % h])
        tiles = [(t0, min(128, gn - t0)) for t0 in range(0, gn, 128)]
        for cg in range(D // 256):
            wi = nW % 2
            nW += 1
            for h in range(8):
                S.dma("pool", wsl[wi][:, h * 4:(h + 1) * 4, :], wov[:, h * 4:(h + 1) * 4, cg * 256:(cg + 1) * 256], writes=["wsl%d_%d" % (wi, h)])
            for ti, (t0, P) in enumerate(tiles):
                pb = (cg * 2 + ti) % 4
                for k in range(32):
                    S.op("pe", lambda e, pb=pb, k=k, t0=t0, P=P, wi=wi: e.matmul(ps[pb][0:P, 0:256], oTg[:, k, t0:t0 + P], wsl[wi][:, k, :], start=(k == 0), stop=(k == 31)),
                         reads=["oTg_%d" % (k // 4), "wsl%d_%d" % (wi, k // 4)], writes=[PK(pb)])
                xi = (cg * 2 + ti) % 2
                S.dma("sp", xs[xi][0:P, :], x[g0 + t0:g0 + t0 + P, cg * 256:(cg + 1) * 256], writes=["xs%d" % xi])
                S.op("dve", lambda e, pb=pb, xi=xi, P=P, cg=cg: e.tensor_tensor(t2[xi][0:P, :], ps[pb][0:P, 0:256], g1b[0:P, cg * 256:(cg + 1) * 256], ALU.mult),
                     reads=[PK(pb)] + BK4("g1b"), writes=["t2%d" % xi])
                S.op("pool", lambda e, xi=xi, P=P, cg=cg, ti=ti: e.tensor_tensor(xm[ti][0:P, cg * 256:(cg + 1) * 256], t2[xi][0:P, :], xs[xi][0:P, :], ALU.add),
                     reads=["t2%d" % xi, "xs%d" % xi], writes=["xm%d_%d" % (ti, cg)])
        for ti, (t0, P) in enumerate(tiles):
            r0 = g0 + t0
            kxm = ["xm%d_%d" % (ti, cg) for cg in range(D // 256)]
            for h in range(2):
                S.dma("act", xmid[r0:r0 + P, h * 2048:(h + 1) * 2048], xm[ti][0:P, h * 2048:(h + 1) * 2048], reads=kxm)
            emit_modnorm(S, xm[ti][0:P, :], P, gsb[0:P, :], shb[0:P, :], tmp[0:P, :], tmp[0:P, :], hbf[0:P, :], stt[0:P, :],
                         kxm, BK4("gsb"), BK4("shb"), ["tmp"], ["tmp"], ["hbf"], ["stt"], eng2="pool")
            S.op("act", lambda e, P=P: e.activation(out=hbf[0:P, :], in_=tmp[0:P, :], func=AF.Copy), reads=["tmp"], writes=["hbf"])
            S.dma("act", h2o[r0:r0 + P, :], hbf[0:P, :], reads=["hbf"])
            for q4 in range(8):
                pb = 4 + q4 % 2
                for s4 in range(4):
                    k = q4 * 4 + s4
                    S.op("pe", lambda e, pb=pb, s4=s4, k=k, P=P: e.transpose(ps[pb][:, s4 * 128:s4 * 128 + P], tmp[0:P, k * 128:(k + 1) * 128], ident[0:P, 0:P]),
                         reads=["tmp", "cs"], writes=[PK(pb)])
                eng = "act" if q4 % 2 == 0 else "dve"
                if eng == "act":
                    S.op("act", lambda e, pb=pb, q4=q4, P=P: e.activation(out=h2T[:, q4 * 4:(q4 + 1) * 4, 0:P], in_=ps[pb].rearrange("p (a b) -> p a b", a=4)[:, :, 0:P], func=AF.Copy),
                         reads=[PK(pb)], writes=["h2T_%d" % q4])
                else:
                    S.op("dve", lambda e, pb=pb, q4=q4, P=P: e.tensor_copy(h2T[:, q4 * 4:(q4 + 1) * 4, 0:P], ps[pb].rearrange("p (a b) -> p a b", a=4)[:, :, 0:P]),
                         reads=[PK(pb)], writes=["h2T_%d" % q4])
            for k in range(32):
                S.op("pe", lambda e, k=k, P=P: e.matmul(ps[6][0:P, 0:36], h2T[:, k, 0:P], rtt[:, k, :], start=(k == 0), stop=(k == 31)),
                     reads=["h2T_%d" % (k // 4), "rtt"], writes=[PK(6)])
            route_tile(P, r0)
    return cx.finish()


def build_C(CAP):
    cx = Ctx()
    S = cx.S
    NE = 4
    hs = cx.din("hs", [NE, D, CAP], BF16)
    wg = cx.din("wg", [NE, D, 512], F32)
    wu = cx.din("wu", [NE, D, 512], F32)
    wd = cx.din("wd", [NE, 512, D], F32)
    y = cx.dout("y", [NE, D, CAP], BF16)
    ps = cx.psum()
    PK = lambda i: "ps%d" % i
    Wg = cx.sb("Wg", [128, 32, 512], BF16)
    Wu = cx.sb("Wu", [128, 32, 512], BF16)
    Wd = cx.sb("Wd", [128, 4, D], BF16)
    hb = [cx.sb("hsb%d" % i, [128, 32, 512], BF16) for i in range(2)]
    act = cx.sb("actT", [128, 4, 512], BF16)
    sa = [cx.sb("sa%d" % i, [128, 512], F32) for i in range(2)]
    ysb = cx.sb("ysb", [128, 32, 512], BF16)
    tiles = [(s0, min(512, CAP - s0)) for s0 in range(0, CAP, 512)]
    nh = 0
    npb = 0
    for e_ in range(NE):
        for h in range(8):
            S.dma("pool", Wg[:, h * 4:(h + 1) * 4, :], wg[e_].rearrange("(c p) n -> p c n", p=128)[:, h * 4:(h + 1) * 4, :], writes=["Wg_%d" % h])
        for h in range(8):
            S.dma("pool", Wu[:, h * 4:(h + 1) * 4, :], wu[e_].rearrange("(c p) n -> p c n", p=128)[:, h * 4:(h + 1) * 4, :], writes=["Wu_%d" % h])
        for h in range(8):
            S.dma("pool", Wd[:, :, h * 512:(h + 1) * 512], wd[e_].rearrange("(c p) n -> p c n", p=128)[:, :, h * 512:(h + 1) * 512], writes=["Wd_%d" % h])
        for (s0, N) in tiles:
            hi = nh % 2
            nh += 1
            for h in range(8):
                S.dma("sp", hb[hi][:, h * 4:(h + 1) * 4, 0:N], hs[e_].rearrange("(c p) s -> p c s", p=128)[:, h * 4:(h + 1) * 4, s0:s0 + N], writes=["hsb%d_%d" % (hi, h)])
            for fc in range(4):
                pa, pu = (npb % 2) * 2, (npb % 2) * 2 + 1
                npb += 1
                for (W_, pk_, wn) in ((Wg, pa, "Wg"), (Wu, pu, "Wu")):
                    for k in range(32):
                        S.op("pe", lambda e, W_=W_, pk_=pk_, k=k, fc=fc, hi=hi, N=N: e.matmul(ps[pk_][:, 0:N], W_[:, k, fc * 128:(fc + 1) * 128], hb[hi][:, k, 0:N],
                                                                                              start=(k == 0), stop=(k == 31)),
                             reads=["%s_%d" % (wn, k // 4), "hsb%d_%d" % (hi, k // 4)], writes=[PK(pk_)])
                si = fc % 2
                S.op("act", lambda e, si=si, pa=pa, N=N: e.activation(out=sa[si][:, 0:N], in_=ps[pa][:, 0:N], func=AF.Silu), reads=[PK(pa)], writes=["sa%d" % si])
                S.op("dve", lambda e, si=si, pu=pu, fc=fc, N=N: e.tensor_tensor(act[:, fc, 0:N], sa[si][:, 0:N], ps[pu][:, 0:N], ALU.mult),
                     reads=["sa%d" % si, PK(pu)], writes=["act_%d" % fc])
            for dc in range(32):
                pb = 4 + dc % 4
                for fc in range(4):
                    S.op("pe", lambda e, pb=pb, fc=fc, dc=dc, N=N: e.matmul(ps[pb][:, 0:N], Wd[:, fc, dc * 128:(dc + 1) * 128], act[:, fc, 0:N], start=(fc == 0), stop=(fc == 3)),
                         reads=["Wd_%d" % (dc // 4), "act_%d" % fc], writes=[PK(pb)])
                if dc % 2 == 0:
                    S.op("act", lambda e, pb=pb, dc=dc, N=N: e.activation(out=ysb[:, dc, 0:N], in_=ps[pb][:, 0:N], func=AF.Copy), reads=[PK(pb)], writes=["ysb_%d" % (dc // 4)])
                else:
                    S.op("dve", lambda e, pb=pb, dc=dc, N=N: e.tensor_copy(ysb[:, dc, 0:N], ps[pb][:, 0:N]), reads=[PK(pb)], writes=["ysb_%d" % (dc // 4)])
            for h in range(8):
                S.dma("act", y[e_].rearrange("(c p) s -> p c s", p=128)[:, h * 4:(h + 1) * 4, s0:s0 + N], ysb[:, h * 4:(h + 1) * 4, 0:N], reads=["ysb_%d" % h])
    return cx.finish()


def _cst_B():
    c = np.zeros((128, 160), np.float32)
    c[:, :128] = np.eye(128, dtype=np.float32)
    c[:, 128:160] = np.arange(32, dtype=np.float32)[None]
    return c


def forward(p, S_, depth, log=print):
    import time
    t00 = time.time()
    x = p["x"]
    B = x.shape[0]
    TOKL = S_ // 4
    TOKCF = CTX // 4
    mod = run_mod(p["c"], p["c_ctx"], p["w_mod"], p["b_mod"])
    log("mod done %.1f" % (time.time() - t00))
    mv = lambda l, k: mod[l][:, k * D:(k + 1) * D]
    seg2 = lambda l, k, b: np.ascontiguousarray(np.stack([mv(l, k)[b], mv(l, k)[2]]))
    xs = []
    for i in range(NCORES):
        b, j = i // 4, i % 4
        xs.append(np.ascontiguousarray(np.concatenate([x[b, j * TOKL:(j + 1) * TOKL], p["ctx"][b, j * TOKCF:(j + 1) * TOKCF]], 0)))
    ncD0 = build_D(TOKL, TOKCF, False, True)
    res = _run(ncD0, [dict(x=xs[i], nm=p["norm_mix"][0], sc=seg2(0, 1, i // 4), sh=seg2(0, 0, i // 4)) for i in range(NCORES)])
    hs_tok = [r["h"] for r in res]
    log("norm0 done %.1f" % (time.time() - t00))
    cstB = _cst_B()
    for l in range(depth):
        with_ctx = l < depth - 1
        TOKC = TOKCF if with_ctx else 0
        hT_b = []
        for b in range(B):
            lat = np.concatenate([hs_tok[b * 4 + j][:TOKL] for j in range(4)], 0)
            cxt = np.concatenate([hs_tok[b * 4 + j][TOKL:TOKL + TOKCF] for j in range(4)], 0)
            hT_b.append(np.ascontiguousarray(np.concatenate([lat, cxt], 0).T))
        lam_init, bargs, imsA = prep_A(p, l, S_, hT_b)
        ncA = build_A(S_, with_ctx, lam_init, *bargs)
        resA = _run(ncA, imsA)
        log("L%d A done %.1f" % (l, time.time() - t00))
        imsB = []
        for i in range(NCORES):
            b, j = i // 4, i % 4
            full = np.empty((D, TOKL + TOKC), NPBF)
            cols = list(range(j * TOKL, (j + 1) * TOKL)) + ([S_ + j * TOKCF + t for t in range(TOKCF)] if with_ctx else [])
            for jj in range(4):
                o = resA[b * 4 + jj]["oT"]
                for g in range(4):
                    for hh in range(2):
                        r0 = g * 1024 + (2 * jj + hh) * 128
                        full[r0:r0 + 128] = o[(2 * g + hh) * 128:(2 * g + hh + 1) * 128][:, cols]
            xi = xs[i][:TOKL + TOKC]
            imsB.append(dict(oTo=full, x=np.ascontiguousarray(xi), wo=p["w_out"][l], g1=seg2(l, 2, b), nm=p["norm_ffn"][l], sc=seg2(l, 4, b), sh=seg2(l, 3, b),
                             rt=np.ascontiguousarray(np.concatenate([p["router_group"][l], p["router_expert"][l]], 1)), cst=cstB))
        ncB = build_B(TOKL, TOKC)
        resB = _run(ncB, imsB)
        log("L%d B done %.1f" % (l, time.time() - t00))
        TOK = TOKL + TOKC
        h2 = np.concatenate([r["h2"] for r in resB], 0)
        route = np.concatenate([r["route"] for r in resB], 0)
        eid = np.rint(route[:, 0:2]).astype(np.int64)
        eid = np.clip(eid, 0, 31)
        T = h2.shape[0]
        flat_e = eid.reshape(-1)
        order = np.argsort(flat_e, kind="stable")
        counts = np.bincount(flat_e, minlength=32)
        CAP = int(max(128, -(-counts.max() // 128) * 128))
        starts = np.cumsum(counts) - counts
        hsE = np.zeros((32, CAP, D), NPBF)
        slot_of = np.empty(2 * T, np.int64)
        for e_ in range(32):
            idx = order[starts[e_]:starts[e_] + counts[e_]]
            hsE[e_, :counts[e_]] = h2[idx // 2]
            slot_of[idx] = np.arange(counts[e_])
        imsC = []
        for i in range(NCORES):
            sl = slice(4 * i, 4 * i + 4)
            imsC.append(dict(hs=np.ascontiguousarray(hsE[sl].transpose(0, 2, 1)), wg=p["w_gate"][l][sl], wu=p["w_up"][l][sl], wd=p["w_down"][l][sl]))
        ncC = build_C(CAP)
        resC = _run(ncC, imsC)
        log("L%d C done (CAP %d) %.1f" % (l, CAP, time.time() - t00))
        yE = np.concatenate([r["y"].transpose(0, 2, 1) for r in resC], 0)
        ysel = yE[flat_e, slot_of].reshape(T, 2, D)
        do_norm = l < depth - 1
        imsD = []
        for i in range(NCORES):
            b = i // 4
            sl = slice(i * TOK, (i + 1) * TOK)
            dd = dict(x=resB[i]["xmid"], ya=np.ascontiguousarray(ysel[sl, 0]), yb=np.ascontiguousarray(ysel[sl, 1]),
                      wts=np.ascontiguousarray(route[sl, 2:4]), g2=seg2(l, 5, b))
            if do_norm:
                dd.update(nm=p["norm_mix"][l + 1], sc=seg2(l + 1, 1, b), sh=seg2(l + 1, 0, b))
            imsD.append(dd)
        ncDl = build_D(TOKL, TOKC, True, do_norm)
        resD = _run(ncDl, imsD)
        log("L%d D done %.1f" % (l, time.time() - t00))
        xs = [r["xo"] for r in resD]
        if do_norm:
            hs_tok = [r["h"] for r in resD]
    out = np.empty((B, S_, D), np.float32)
    for i in range(NCORES):
        b, j = i // 4, i % 4
        out[b, j * TOKL:(j + 1) * TOKL] = xs[i][:TOKL]
    return out


def kernel(**inputs):
    p = {k: np.asarray(v) for k, v in inputs.items()}
    return forward(p, p["x"].shape[1], DEPTH, log=lambda *a: print("[kernel]", *a, flush=True))
```

```python
import numpy as np
import concourse.bass as bass
import concourse.mybir as mybir
from concourse.bass_utils import run_bass_kernel_spmd

F32 = mybir.dt.float32
BF16 = mybir.dt.bfloat16
I32 = mybir.dt.int32
ALU = mybir.AluOpType
AF = mybir.ActivationFunctionType
AX = mybir.AxisListType


class Sched:
    COMPUTE = ("pe", "act", "dve", "pool")

    def __init__(self, nc, n_dma_sems=12):
        self.nc = nc
        self.ops = []
        self.last_w = {}
        self.readers = {}
        self.n_dma_sems = n_dma_sems
        self.dma_count = {"sp": 0, "pool": 0, "act": 0}
        self.bar_idx = None

    def _add(self, eng, fn, reads, writes, is_dma):
        idx = len(self.ops)
        deps = set()
        if self.bar_idx is not None:
            deps.add(self.bar_idx)
        for k in reads:
            w = self.last_w.get(k)
            if w is not None:
                deps.add(w)
        for k in writes:
            w = self.last_w.get(k)
            if w is not None:
                deps.add(w)
            for r in self.readers.get(k, ()):
                deps.add(r)
        for k in writes:
            self.last_w[k] = idx
            self.readers[k] = []
        for k in reads:
            self.readers.setdefault(k, []).append(idx)
        op = dict(eng=eng, fn=fn, deps=deps, dma=is_dma, sig=False)
        if is_dma:
            j = self.dma_count[eng]
            self.dma_count[eng] += 1
            op["dma_j"] = j
        self.ops.append(op)
        return idx

    def op(self, eng, fn, reads=(), writes=()):
        return self._add(eng, fn, tuple(reads), tuple(writes), False)

    def dma(self, q, out, in_, reads=(), writes=(), **kw):
        return self._add(q, lambda e: e.dma_start(out=out, in_=in_, **kw), tuple(reads), tuple(writes), True)

    def dma_fn(self, q, fn, reads=(), writes=()):
        return self._add(q, fn, tuple(reads), tuple(writes), True)

    def emit(self):
        nc = self.nc
        ops = self.ops
        N = self.n_dma_sems
        for i, op in enumerate(ops):
            best = {}
            dl = []
            for d in op["deps"]:
                o = ops[d]
                if o["dma"]:
                    dl.append(d)
                else:
                    if o["eng"] == op["eng"] and not op["dma"] and op["eng"] == "pe":
                        continue
                    if o["eng"] not in best or best[o["eng"]] < d:
                        best[o["eng"]] = d
            op["cdeps"] = best
            op["ddeps"] = dl
            for d in best.values():
                ops[d]["sig"] = True
        cnt = {e: 0 for e in self.COMPUTE}
        for op in ops:
            if not op["dma"] and op["sig"]:
                cnt[op["eng"]] += 1
                op["cnt"] = cnt[op["eng"]]
        import contextlib
        with contextlib.ExitStack() as st:
            csem = {e: st.enter_context(nc.semaphore("s_" + e)) for e in self.COMPUTE}
            dsem = {}
            for q, c in self.dma_count.items():
                if c > 0:
                    dsem[q] = [st.enter_context(nc.semaphore("d_%s%d" % (q, i))) for i in range(min(N, c))]
            block = st.enter_context(nc.Block())
            streams = {}
            for i, op in enumerate(ops):
                streams.setdefault(op["eng"], []).append(i)
            engmap = {"pe": "tensor", "act": "scalar", "dve": "vector", "pool": "gpsimd", "sp": "sync"}

            def run_stream(ename, e):
                waited = {}

                def wait(sem, val, key):
                    if waited.get(key, 0) >= val:
                        return
                    waited[key] = val
                    e.wait_ge(sem, val)

                for i in streams.get(ename, []):
                    op = ops[i]
                    for de, d in op["cdeps"].items():
                        wait(csem[de], ops[d]["cnt"], ("c", de))
                    for d in op["ddeps"]:
                        o = ops[d]
                        j = o["dma_j"]
                        wait(dsem[o["eng"]][j % N], 16 * (j // N + 1), ("d", o["eng"], j % N))
                    if op["dma"]:
                        j = op["dma_j"]
                        if j >= N:
                            wait(dsem[ename][j % N], 16 * (j // N), ("d", ename, j % N))
                        ins = op["fn"](e)
                        ins.then_inc(dsem[ename][j % N], 16)
                    else:
                        ins = op["fn"](e)
                        if op["sig"]:
                            ins.then_inc(csem[ename], 1)
                if ename == "sp":
                    for q, c in self.dma_count.items():
                        for s in range(min(N, c)):
                            last_j = ((c - 1 - s) // N) * N + s
                            wait(dsem[q][s], 16 * (last_j // N + 1), ("d", q, s))
                    for ce in self.COMPUTE:
                        if cnt[ce] > 0:
                            wait(csem[ce], cnt[ce], ("c", ce))

            for ename in ("sp", "pe", "act", "dve", "pool"):
                if ename in streams or ename == "sp":
                    getattr(block, engmap[ename])(lambda e, en=ename: run_stream(en, e))

    def barrier(self):
        keys = list(self.last_w.keys()) + list(self.readers.keys())
        self.bar_idx = self.op("dve", lambda e: e.engine_nop() if hasattr(e, "engine_nop") else e.nop(), reads=keys, writes=["__bar__"] + keys)


D = 4096
HD = 128
DEPTH = 2
CTX = 256
GRID_W = 64
EPS = 1e-6
NEG = -30000.0
NCORES = 8
import contextlib
import ml_dtypes
NPBF = ml_dtypes.bfloat16


def _run(nc, in_maps):
    res = run_bass_kernel_spmd(nc, in_maps, core_ids=list(range(NCORES)))
    return res.results


class Ctx:
    def __init__(self):
        self.nc = bass.Bass("TRN2", target_bir_lowering=False)
        self.st = contextlib.ExitStack()
        self.S = Sched(self.nc)
        self.ps = None

    def din(self, name, shape, dt):
        return self.nc.dram_tensor(name, list(shape), dt, kind="ExternalInput").ap()

    def dout(self, name, shape, dt):
        return self.nc.dram_tensor(name, list(shape), dt, kind="ExternalOutput").ap()

    def dscr(self, name, shape, dt):
        return self.nc.dram_tensor(name, list(shape), dt).ap()

    def sb(self, name, shape, dt):
        return self.st.enter_context(self.nc.sbuf_tensor(name, list(shape), dt))[:]

    def psum(self):
        if self.ps is None:
            self.ps = [self.st.enter_context(self.nc.psum_tensor("ps%d" % i, [128, 512], F32))[:] for i in range(8)]
        return self.ps

    def finish(self):
        self.S.emit()
        self.st.close()
        return self.nc


def emit_modnorm(S, x_ap, P, gs_ap, sh_ap, out_ap, tmp_f, junk_bf, st_ap, kx, kgs, ksh, kout, ktmp, kjunk, kst, eng2="pool"):
    S.op("act", lambda e: e.activation(out=junk_bf, in_=x_ap, func=AF.Square, accum_out=st_ap[:, 0:1]),
         reads=kx, writes=kjunk + kst)
    S.op("dve", lambda e: e.tensor_scalar(st_ap[:, 1:2], st_ap[:, 0:1], 1.0 / D, EPS, ALU.mult, ALU.add),
         reads=kst, writes=kst)
    S.op("act", lambda e: e.activation(out=st_ap[:, 2:3], in_=st_ap[:, 1:2], func=AF.Sqrt), reads=kst, writes=kst)
    S.op("dve", lambda e: e.reciprocal(st_ap[:, 3:4], st_ap[:, 2:3]), reads=kst, writes=kst)
    S.op("dve", lambda e: e.scalar_tensor_tensor(tmp_f, x_ap, st_ap[:, 3:4], gs_ap, ALU.mult, ALU.mult),
         reads=kx + kst + kgs, writes=ktmp)
    S.op(eng2, lambda e: e.tensor_tensor(out_ap, tmp_f, sh_ap, ALU.add), reads=ktmp + ksh, writes=kout)


def build_mod():
    cx = Ctx()
    S = cx.S
    CW = 6 * D // NCORES
    cinT = cx.din("cinT", [D, 3], F32)
    wm = cx.din("wm", [DEPTH, D, CW], F32)
    bm = cx.din("bm", [DEPTH, 3, CW], F32)
    out = cx.dout("mod", [DEPTH, 3, CW], F32)
    ps = cx.psum()
    ct = cx.sb("ct", [128, 32, 3], F32)
    wt = [cx.sb("wt%d" % i, [128, 32, 512], F32) for i in range(2)]
    bt = cx.sb("bt", [3, DEPTH, CW], F32)
    ot = cx.sb("ot", [3, DEPTH, CW], F32)
    S.dma("sp", ct, cinT.rearrange("(c p) r -> p c r", p=128), writes=["ct"])
    S.dma("sp", bt, bm.rearrange("l r c -> r l c"), writes=["bt"])
    S.op("act", lambda e: e.activation(out=ct, in_=ct, func=AF.Silu), reads=["ct"], writes=["ct"])
    n = 0
    for l in range(DEPTH):
        wv = wm[l].rearrange("(c p) n -> p c n", p=128)
        for s in range(CW // 512):
            w = wt[n % 2]
            kw = "wt%d" % (n % 2)
            for h in range(8):
                S.dma("sp" if h % 2 == 0 else "act", w[:, h * 4:(h + 1) * 4, :], wv[:, h * 4:(h + 1) * 4, s * 512:(s + 1) * 512],
                      writes=[kw + "_%d" % h])
            p = ps[n % 4]
            kp = "ps%d" % (n % 4)
            for k in range(32):
                S.op("pe", lambda e, p=p, w=w, k=k: e.matmul(p[0:3, :], ct[:, k, :], w[:, k, :], start=(k == 0), stop=(k == 31)),
                     reads=["ct", kw + "_%d" % (k // 4)], writes=[kp])
            S.op("dve", lambda e, p=p, l=l, s=s: e.tensor_tensor(ot[:, l, s * 512:(s + 1) * 512], p[0:3, :], bt[:, l, s * 512:(s + 1) * 512], ALU.add),
                 reads=[kp, "bt"], writes=["ot"])
            n += 1
    S.dma("sp", out.rearrange("l r c -> r l c"), ot, reads=["ot"])
    return cx.finish()


def run_mod(c, c_ctx, w_mod, b_mod):
    CW = 6 * D // NCORES
    cinT = np.ascontiguousarray(np.concatenate([c, c_ctx[None]], 0).T)
    nc = build_mod()
    ims = []
    for i in range(NCORES):
        sl = slice(i * CW, (i + 1) * CW)
        ims.append({"cinT": cinT, "wm": np.ascontiguousarray(w_mod[:, :, sl]),
                    "bm": np.ascontiguousarray(np.broadcast_to(b_mod[:, None, sl], (DEPTH, 3, CW)))})
    res = _run(nc, ims)
    return np.concatenate([r["mod"] for r in res], axis=2)


def load_bcast(S, q, dst, src_row, key, also=()):
    for h in range(4):
        S.dma(q, dst[:, h * 1024:(h + 1) * 1024], src_row[h * 1024:(h + 1) * 1024].partition_broadcast(128), writes=[key + "_%d" % h] + list(also))


BK4 = lambda k: [k + "_%d" % h for h in range(4)]


def build_D(TOKL, TOKC, do_combine, do_norm):
    cx = Ctx()
    S = cx.S
    TOK = TOKL + TOKC
    x = cx.din("x", [TOK, D], F32)
    if do_combine:
        ya = cx.din("ya", [TOK, D], BF16)
        yb = cx.din("yb", [TOK, D], BF16)
        wts = cx.din("wts", [TOK, 2], F32)
        g2 = cx.din("g2", [2, D], F32)
        xo = cx.dout("xo", [TOK, D], F32)
    if do_norm:
        nm = cx.din("nm", [D], F32)
        sc = cx.din("sc", [2, D], F32)
        sh = cx.din("sh", [2, D], F32)
        ho = cx.dout("h", [TOK, D], BF16)
    xt = [cx.sb("xt%d" % i, [128, D], F32) for i in range(2)]
    tmp = cx.sb("tmp", [128, D], F32)
    if do_combine:
        yat = [cx.sb("yat%d" % i, [128, D], BF16) for i in range(2)]
        ybt = [cx.sb("ybt%d" % i, [128, D], BF16) for i in range(2)]
        wt = [cx.sb("wtt%d" % i, [128, 2], F32) for i in range(2)]
        g2b = cx.sb("g2b", [128, D], F32)
    if do_norm:
        gsb = cx.sb("gsb", [128, D], F32)
        shb = cx.sb("shb", [128, D], F32)
        nmb = cx.sb("nmb", [128, D], F32)
        hb = [cx.sb("hb%d" % i, [128, D], BF16) for i in range(2)]
        junk = cx.sb("junk", [128, D], BF16)
        stt = [cx.sb("stt%d" % i, [128, 4], F32) for i in range(2)]
        load_bcast(S, "act", nmb, nm, "nmb")
    n = 0
    for seg in range(2):
        rows = TOKL if seg == 0 else TOKC
        base = 0 if seg == 0 else TOKL
        if rows == 0:
            continue
        if do_combine:
            load_bcast(S, "act", g2b, g2[seg], "g2b")
        if do_norm:
            load_bcast(S, "act", gsb, sc[seg], "gsb")
            load_bcast(S, "act", shb, sh[seg], "shb")
            S.op("dve", lambda e: e.scalar_tensor_tensor(gsb, gsb, 1.0, nmb, ALU.add, ALU.mult),
                 reads=BK4("gsb") + BK4("nmb"), writes=BK4("gsb"))
        r0 = 0
        while r0 < rows:
            P = min(128, rows - r0)
            i = n % 2
            xa = xt[i][0:P, :]
            kx = "xt%d" % i
            for h in range(2):
                S.dma("sp", xt[i][0:P, h * 2048:(h + 1) * 2048], x[base + r0:base + r0 + P, h * 2048:(h + 1) * 2048], writes=[kx])
            if do_combine:
                S.dma("sp", yat[i][0:P, :], ya[base + r0:base + r0 + P, :], writes=["ya%d" % i])
                S.dma("sp", ybt[i][0:P, :], yb[base + r0:base + r0 + P, :], writes=["yb%d" % i])
                S.dma("sp", wt[i][0:P, :], wts[base + r0:base + r0 + P, :], writes=["wt%d" % i])
                S.op("dve", lambda e, i=i, P=P: e.tensor_scalar(tmp[0:P, :], yat[i][0:P, :], wt[i][0:P, 0:1], None, ALU.mult),
                     reads=["ya%d" % i, "wt%d" % i], writes=["tmp"])
                S.op("dve", lambda e, i=i, P=P: e.scalar_tensor_tensor(tmp[0:P, :], ybt[i][0:P, :], wt[i][0:P, 1:2], tmp[0:P, :], ALU.mult, ALU.add),
                     reads=["yb%d" % i, "wt%d" % i, "tmp"], writes=["tmp"])
                S.op("pool", lambda e, P=P: e.tensor_tensor(tmp[0:P, :], tmp[0:P, :], g2b[0:P, :], ALU.mult),
                     reads=["tmp"] + BK4("g2b"), writes=["tmp"])
                S.op("pool", lambda e, xa=xa, P=P: e.tensor_tensor(xa, xa, tmp[0:P, :], ALU.add), reads=["tmp", kx], writes=[kx])
                for h in range(2):
                    S.dma("act", xo[base + r0:base + r0 + P, h * 2048:(h + 1) * 2048], xt[i][0:P, h * 2048:(h + 1) * 2048], reads=[kx])
            if do_norm:
                emit_modnorm(S, xa, P, gsb[0:P, :], shb[0:P, :], hb[i][0:P, :], tmp[0:P, :], junk[0:P, :], stt[i][0:P, :],
                             [kx], BK4("gsb"), BK4("shb"), ["hb%d" % i], ["tmp"], ["junk"], ["st%d" % i])
                S.dma("act", ho[base + r0:base + r0 + P, :], hb[i][0:P, :], reads=["hb%d" % i])
            r0 += P
            n += 1
    return cx.finish()


class Arena:
    def __init__(self, ap, n):
        self.ap, self.n, self.off = ap, n, 0

    def reset(self):
        self.off = 0

    def _shape(self, v, shape):
        if len(shape) == 1:
            return v
        if len(shape) == 2:
            return v.rearrange("p (a b) -> p a b", a=shape[0])
        return v.rearrange("p (a b c) -> p a b c", a=shape[0], b=shape[1])

    def bf(self, *shape):
        n = int(np.prod(shape))
        n2 = n + (n % 2)
        v = self.ap[:, self.off:self.off + n]
        self.off += n2
        assert self.off <= self.n, "arena overflow %d" % self.off
        return self._shape(v, shape)

    def f32(self, *shape):
        n = int(np.prod(shape)) * 2
        v = self.ap[:, self.off:self.off + n].bitcast(F32)
        self.off += n
        assert self.off <= self.n, "arena overflow %d" % self.off
        return self._shape(v, shape)


FM_KIND = ["nrA", "nrA", "nrA", "nB", "nB", "nB", "nB", "nrC", "nrC", "nrC", "nrC", "scale", "scale", "copy", "copy", "silu", "silu"]
FM_GAIN = [0, 0, 1, 2, 2, 3, 3, 4, 4, 5, 5, -1, -1, -1, -1, -1, -1]
PASSES = [(list(range(0, 7)), [0, 1, 2]), (list(range(7, 11)), [3, 4]), (list(range(11, 17)), [5, 6, 7, 8])]


def build_A(S_, with_ctx, lam_init, nBpat, bpat_of_block, b_rlo_of_block, phases=("P", "A", "B", "C", "D")):
    cx = Ctx()
    S = cx.S
    NT = S_ + CTX
    NLB = S_ // 512
    blocks = [(i * 512, 512, True) for i in range(NLB)] + [(S_, CTX, False)]
    NKT = NT // 128
    NLT = S_ // 128
    hT = cx.din("hT", [D, NT], BF16)
    wfm = cx.din("wfm", [D, 17 * 128], F32)
    wtm = cx.din("wtm", [D, 9 * 128], F32)
    gains = cx.din("gains", [128, 8], F32)
    ropeA = cx.din("ropeA", [2, 128, S_], F32)
    ropeC = cx.din("ropeC", [2, 128, S_], F32)
    rmat = cx.din("rmat", [4, 128, 128], F32)
    maskA = cx.din("maskA", [6, 128, 512], F32)
    biasB = cx.din("biasB", [2, nBpat, 8, 128, 512], F32)
    sinks = cx.din("sinks", [128, 2], F32)
    lamc = cx.din("lamc", [128, 4, 64], F32)
    dtab = cx.din("dtab", [6, 128, 128], F32)
    dcol = cx.din("dcol", [128, 2], F32)
    lgd = cx.din("lgd", [128, 4], F32)
    oT = cx.dout("oT", [8 * 128, NT], BF16)
    import os
    _dbg = bool(os.environ.get("KDBG"))
    QT = (cx.dout if _dbg else cx.dscr)("QT", [17, 128, NT], BF16)
    VT = (cx.dout if _dbg else cx.dscr)("VT", [9, NT, 128], BF16)
    ps = cx.psum()
    PK = lambda i: "ps%d" % i

    cm = cx.sb("cm", [128, 4, 128], BF16)
    for i in range(4):
        S.dma("pool", cm[:, i, :], rmat[i], writes=["cm"])
    RA, RC, ONES, BONES = cm[:, 0, :], cm[:, 1, :], cm[:, 2, :], cm[:, 3, :]
    gn = cx.sb("gn", [128, 8], F32)
    S.dma("sp", gn, gains, writes=["gn"])
    sm = cx.sb("sm", [128, 32], F32)
    S.dma("sp", sm[:, 0:2], sinks, writes=["sm_sink"])
    S.op("act", lambda e: e.activation(out=sm[:, 2:4], in_=sm[:, 0:2], func=AF.Exp), reads=["sm_sink"], writes=["sm_esink"])
    ARN = 96 * 1024
    art = cx.sb("arena", [128, ARN], BF16)
    ar = Arena(art, ARN)

    def rstd_from(ss_ps, N, dh, dst, kps, kdst):
        S.op("dve", lambda e: e.tensor_scalar(dst[:, 0:N], ss_ps[:, 0:N], 1.0 / dh, EPS, ALU.mult, ALU.add), reads=[kps], writes=[kdst])
        S.op("act", lambda e: e.activation(out=dst[:, 0:N], in_=dst[:, 0:N], func=AF.Sqrt), reads=[kdst], writes=[kdst])
        S.op("dve", lambda e: e.reciprocal(dst[:, 0:N], dst[:, 0:N]), reads=[kdst], writes=[kdst])

    if "P" in phases:
        ar.reset()
        hb = [ar.bf(32, 512) for _ in range(2)]
        wreg = ar.bf(32, 1280)
        sq = [ar.bf(512) for _ in range(2)]
        qn = [ar.bf(512) for _ in range(2)]
        ob = [ar.bf(512) for _ in range(4)]
        tb_ = [ar.bf(512) for _ in range(2)]
        rs = [ar.f32(512) for _ in range(2)]
        t1 = [ar.f32(512) for _ in range(2)]
        t2 = [ar.f32(512) for _ in range(2)]
        rp = [ar.f32(4, 512) for _ in range(2)]
        cnt = dict(hb=0, ep=0, ob=0, tm=0, mm=0)
        pendA, pendB = [], []

        def step_pending():
            if pendB:
                pendB.pop(0)()
            if pendA:
                pendA.pop(0)()

        def emit_fm_tile(wi, f, h_, khs, N, t0, bi, lat, rpi):
            kind = FM_KIND[f]
            m = cnt["mm"] % 3
            cnt["mm"] += 1
            pm = ps[m]
            for k in range(32):
                S.op("pe", lambda e, k=k: e.matmul(pm[:, 0:N], wreg[:, k, wi * 128:(wi + 1) * 128], h_[:, k, 0:N], start=(k == 0), stop=(k == 31)),
                     reads=["w%d" % wi, khs[k // 4]], writes=[PK(m)])
            step_pending()

            def epA():
                oi = cnt["ob"] % 4
                cnt["ob"] += 1
                o_ = ob[oi]
                ko = "ob%d" % oi
                store = lambda: S.dma("sp", QT[f][:, t0:t0 + N], o_[:, 0:N], reads=[ko], writes=["QT%d_%d" % (f, bi)])
                if kind in ("scale", "copy", "silu"):
                    fn_ = AF.Silu if kind == "silu" else AF.Copy
                    sc_ = HD ** -0.5 if kind == "scale" else 1.0
                    S.op("act", lambda e: e.activation(out=o_[:, 0:N], in_=pm[:, 0:N], func=fn_, scale=sc_), reads=[PK(m)], writes=[ko])
                    store()
                    return
                ei = cnt["ep"] % 2
                cnt["ep"] += 1
                dh = 64 if kind == "nrC" else 128
                onesm = BONES if kind == "nrC" else ONES
                sq_, qn_, rs_, t1_, t2_ = sq[ei], qn[ei], rs[ei], t1[ei], t2[ei]
                ke = "e%d" % ei
                S.op("act", lambda e: e.activation(out=sq_[:, 0:N], in_=pm[:, 0:N], func=AF.Square), reads=[PK(m)], writes=[ke + "sq"])
                p2 = ps[3 + ei]
                S.op("pe", lambda e: e.matmul(p2[:, 0:N], onesm, sq_[:, 0:N], start=True, stop=True), reads=[ke + "sq", "cm"], writes=[PK(3 + ei)])
                rstd_from(p2, N, dh, rs_, PK(3 + ei), ke + "rs")
                gcol = gn[:, FM_GAIN[f]:FM_GAIN[f] + 1]
                rope = lat and kind in ("nrA", "nrC")
                dst = qn_ if rope else o_
                kd = (ke + "qn") if rope else ko
                S.op("dve", lambda e: e.scalar_tensor_tensor(dst[:, 0:N], pm[:, 0:N], gcol, rs_[:, 0:N], ALU.mult, ALU.mult),
                     reads=[PK(m), "gn", ke + "rs"], writes=[kd])
                if not rope:
                    store()
                    return

                def epB():
                    Rm = RA if kind == "nrA" else RC
                    ci = 0 if kind == "nrA" else 2
                    p3 = ps[5 + ei]
                    S.op("pe", lambda e: e.matmul(p3[:, 0:N], Rm, qn_[:, 0:N], start=True, stop=True), reads=[ke + "qn", "cm"], writes=[PK(5 + ei)])
                    S.op("pool", lambda e: e.tensor_tensor(t1_[:, 0:N], qn_[:, 0:N], rp[rpi][:, ci, 0:N], ALU.mult), reads=[ke + "qn", "rp%d" % rpi], writes=[ke + "t1"])
                    S.op("dve", lambda e: e.tensor_tensor(t2_[:, 0:N], p3[:, 0:N], rp[rpi][:, ci + 1, 0:N], ALU.mult), reads=[PK(5 + ei), "rp%d" % rpi], writes=[ke + "t2"])
                    S.op("pool", lambda e: e.tensor_tensor(o_[:, 0:N], t1_[:, 0:N], t2_[:, 0:N], ALU.add), reads=[ke + "t1", ke + "t2"], writes=[ko])
                    store()
                pendB.append(epB)
            pendA.append(epA)

        for pi, (fms, tms) in enumerate(PASSES):
            nf, nt_ = len(fms), len(tms)
            for wi, f in enumerate(fms):
                for h in range(8):
                    S.dma("pool", wreg[:, h * 4:(h + 1) * 4, wi * 128:(wi + 1) * 128],
                          wfm.rearrange("(c p) n -> p c n", p=128)[:, h * 4:(h + 1) * 4, f * 128:(f + 1) * 128], writes=["w%d" % wi])
            for wi, t in enumerate(tms):
                for h in range(8):
                    S.dma("pool", wreg[:, h * 4:(h + 1) * 4, (nf + wi) * 128:(nf + wi + 1) * 128],
                          wtm.rearrange("(c p) n -> p c n", p=128)[:, h * 4:(h + 1) * 4, t * 128:(t + 1) * 128], writes=["w%d" % (nf + wi)])
            for bi, (t0, N, lat) in enumerate(blocks):
                hbi = cnt["hb"] % 2
                cnt["hb"] += 1
                h_ = hb[hbi]
                kh = "hb%d" % hbi
                for h in range(8):
                    S.dma("sp", h_[:, h * 4:(h + 1) * 4, 0:N], hT.rearrange("(c p) n -> p c n", p=128)[:, h * 4:(h + 1) * 4, t0:t0 + N],
                          writes=[kh + "_%d" % h])
                khs = [kh + "_%d" % h for h in range(8)]
                need_rope = lat and any(FM_KIND[f].startswith("nr") for f in fms)
                rpi = bi % 2
                if need_rope:
                    if pi == 0:
                        S.dma("act", rp[rpi][:, 0, :], ropeA[0][:, t0:t0 + N], writes=["rp%d" % rpi])
                        S.dma("act", rp[rpi][:, 1, :], ropeA[1][:, t0:t0 + N], writes=["rp%d" % rpi])
                    else:
                        S.dma("act", rp[rpi][:, 2, :], ropeC[0][:, t0:t0 + N], writes=["rp%d" % rpi])
                        S.dma("act", rp[rpi][:, 3, :], ropeC[1][:, t0:t0 + N], writes=["rp%d" % rpi])
                for wi, f in enumerate(fms):
                    emit_fm_tile(wi, f, h_, khs, N, t0, bi, lat, rpi)
                ncol = nt_ * 128
                for sub in range(N // 128):
                    m = 7
                    ti = cnt["tm"] % 2
                    cnt["tm"] += 1
                    pm = ps[m]
                    for k in range(32):
                        S.op("pe", lambda e, pm=pm, k=k, h_=h_, sub=sub, ncol=ncol, nf=nf: e.matmul(pm[:, 0:ncol], h_[:, k, sub * 128:(sub + 1) * 128],
                                                                                                   wreg[:, k, nf * 128:nf * 128 + ncol], start=(k == 0), stop=(k == 31)),
                             reads=["w%d" % (nf + wi_) for wi_ in range(nt_)] + [khs[k // 4]], writes=[PK(m)])
                    tb1 = tb_[ti]
                    S.op("act", lambda e, tb1=tb1, pm=pm, ncol=ncol: e.activation(out=tb1[:, 0:ncol], in_=pm[:, 0:ncol], func=AF.Copy),
                         reads=[PK(m)], writes=["tb%d" % ti])
                    for wi_, t in enumerate(tms):
                        S.dma("act", VT[t][t0 + sub * 128:t0 + (sub + 1) * 128, :], tb1[:, wi_ * 128:(wi_ + 1) * 128], reads=["tb%d" % ti],
                              writes=["VT%d_%d" % (t, bi)])
        while pendA or pendB:
            step_pending()
        S.barrier()
    QK = lambda f: ["QT%d_%d" % (f, bi) for bi in range(len(blocks))]
    VK = lambda t: ["VT%d_%d" % (t, bi) for bi in range(len(blocks))]

    ar.reset()
    kT = ar.bf(NT)
    qT = ar.bf(NT)
    vv = ar.bf(NKT, 128)
    Pt = [ar.bf(512) for _ in range(6)]
    tf = [ar.f32(512) for _ in range(2)]
    rl = [ar.f32(512) for _ in range(2)]
    on = [ar.f32(512) for _ in range(3)]
    osb = [ar.bf(512) for _ in range(2)]
    state = dict(p=0, s=0, o=0)

    def load_fm(dst, f, key, q="sp"):
        for c in range(0, NT, 2048):
            n = min(2048, NT - c)
            S.dma(q, dst[:, c:c + n], QT[f][:, c:c + n], reads=QK(f), writes=[key])

    def load_v(t, key="vv"):
        for c in range(0, NKT, 4):
            n = min(4, NKT - c)
            S.dma("act", vv[:, c:c + n, :], VT[t][c * 128:(c + n) * 128, :].rearrange("(c p) d -> p c d", p=128), reads=VK(t), writes=[key])

    def attn_block(q_ap, N, keys, scale, o_idx, l_idx, kpart=None, qkey="qT", kkey="kT"):
        nk = len(keys)
        lo, hi = kpart if kpart else (0, 128)
        sis = []

        def qk(i):
            kt = keys[i][0]
            si = state["s"] % 2
            state["s"] += 1
            sis.append(si)
            S.op("pe", lambda e: e.matmul(ps[si][:, 0:N], kT[lo:hi, kt * 128:(kt + 1) * 128], q_ap[lo:hi, 0:N], start=True, stop=True),
                 reads=[kkey, qkey], writes=[PK(si)])

        def rest(i):
            kt, bias, bkey = keys[i]
            si = sis[i]
            ps_s = ps[si]
            pi_ = state["p"] % 6
            state["p"] += 1
            P_ = Pt[pi_]
            kp = "P%d" % pi_
            if bias is not None:
                ti = state["p"] % 2
                tf_ = tf[ti]
                S.op("dve", lambda e: e.scalar_tensor_tensor(tf_[:, 0:N], ps_s[:, 0:N], scale, bias[:, 0:N], ALU.mult, ALU.add),
                     reads=[PK(si), bkey], writes=["tf%d" % ti])
                S.op("act", lambda e: e.activation(out=P_[:, 0:N], in_=tf_[:, 0:N], func=AF.Exp), reads=["tf%d" % ti], writes=[kp])
            else:
                S.op("act", lambda e: e.activation(out=P_[:, 0:N], in_=ps_s[:, 0:N], func=AF.Exp, scale=scale), reads=[PK(si)], writes=[kp])
            S.op("pe", lambda e: e.matmul(ps[o_idx][:, 0:N], vv[:, kt, :], P_[:, 0:N], start=(i == 0), stop=(i == nk - 1)),
                 reads=[kp, "vv"], writes=[PK(o_idx)])
            S.op("pe", lambda e: e.matmul(ps[l_idx][:, 0:N], ONES, P_[:, 0:N], start=(i == 0), stop=(i == nk - 1)),
                 reads=[kp, "cm"], writes=[PK(l_idx)])

        qk(0)
        for i in range(nk):
            if i + 1 < nk:
                qk(i + 1)
            rest(i)

    def attn_block2(q_ap, N, keys, scale):
        nk = len(keys)
        sis = {}
        c2 = [0, 0]

        def qk(i, sub):
            kt = keys[i][0]
            lo, hi = (0, 64) if sub == 0 else (64, 128)
            si = (0 if sub == 0 else 6) + c2[sub] % 2
            c2[sub] += 1
            sis[(i, sub)] = si
            S.op("pe", lambda e: e.matmul(ps[si][:, 0:N], kT[lo:hi, kt * 128:(kt + 1) * 128], q_ap[lo:hi, 0:N], start=True, stop=True),
                 reads=["kT", "qT"], writes=[PK(si)])

        def rest(i, sub):
            kt = keys[i][0]
            si = sis[(i, sub)]
            pi_ = state["p"] % 6
            state["p"] += 1
            P_ = Pt[pi_]
            kp = "P%d" % pi_
            S.op("act", lambda e: e.activation(out=P_[:, 0:N], in_=ps[si][:, 0:N], func=AF.Exp, scale=scale), reads=[PK(si)], writes=[kp])
            S.op("pe", lambda e: e.matmul(ps[2 + sub][:, 0:N], vv[:, kt, :], P_[:, 0:N], start=(i == 0), stop=(i == nk - 1)),
                 reads=[kp, "vv"], writes=[PK(2 + sub)])
            S.op("pe", lambda e: e.matmul(ps[4 + sub][:, 0:N], ONES, P_[:, 0:N], start=(i == 0), stop=(i == nk - 1)),
                 reads=[kp, "cm"], writes=[PK(4 + sub)])

        qk(0, 0)
        qk(0, 1)
        for i in range(nk):
            if i + 1 < nk:
                qk(i + 1, 0)
                qk(i + 1, 1)
            rest(i, 0)
            rest(i, 1)

    def finish_softmax(N, o_idx, l_idx, sink_col, out_ap, kout, f32_out=False):
        ri = state["o"] % 2
        state["o"] += 1
        rl_ = rl[ri]
        kr = "rl%d" % ri
        if sink_col is not None:
            S.op("dve", lambda e: e.tensor_scalar(rl_[:, 0:N], ps[l_idx][:, 0:N], sink_col, None, ALU.add), reads=[PK(l_idx), "sm_esink"], writes=[kr])
            S.op("dve", lambda e: e.reciprocal(rl_[:, 0:N], rl_[:, 0:N]), reads=[kr], writes=[kr])
        else:
            S.op("dve", lambda e: e.reciprocal(rl_[:, 0:N], ps[l_idx][:, 0:N]), reads=[PK(l_idx)], writes=[kr])
        S.op("dve", lambda e: e.tensor_tensor(out_ap[:, 0:N], ps[o_idx][:, 0:N], rl_[:, 0:N], ALU.mult), reads=[PK(o_idx), kr], writes=[kout])

    def store_o(slot, t0, N, src, ksrc):
        S.dma("sp", oT[slot * 128:(slot + 1) * 128, t0:t0 + N], src[:, 0:N], reads=[ksrc], writes=["oT"])

    ctx_keys = [(NLT + i, None, None) for i in range(CTX // 128)]
    qblocks = [(i * 512, 512) for i in range(NLB)]

    if "A" in phases:
        mA = ar.f32(6, 512)
        for i in range(6):
            S.dma("act", mA[:, i, :], maskA[i], writes=["mA"])
        load_fm(kT, 2, "kT")
        load_v(0)
        for hq in range(2):
            load_fm(qT, hq, "qT")
            sc_ = HD ** -0.5
            for (q0, N) in qblocks:
                k_lo, k_hi = max(0, q0 - 128), min(S_, q0 + 640)
                keys = [(k0 // 128, mA[:, (k0 - q0) // 128 + 1, :], "mA") for k0 in range(k_lo, k_hi, 128)] + ctx_keys
                oi, li = 2 + (state["o"] % 2), 4 + (state["o"] % 2)
                attn_block(qT[:, q0:q0 + N], N, keys, sc_, oi, li)
                ob_ = osb[state["o"] % 2]
                ko = "osb%d" % (state["o"] % 2)
                finish_softmax(N, oi, li, sm[:, 2 + hq:3 + hq], ob_, ko)
                store_o(hq, q0, N, ob_, ko)
            if with_ctx:
                N = CTX
                oi, li = 2 + (state["o"] % 2), 4 + (state["o"] % 2)
                attn_block(qT[:, S_:S_ + N], N, ctx_keys, sc_, oi, li)
                ob_ = osb[state["o"] % 2]
                ko = "osb%d" % (state["o"] % 2)
                finish_softmax(N, oi, li, sm[:, 2 + hq:3 + hq], ob_, ko)
                store_o(hq, S_, N, ob_, ko)
        S.barrier()
    base_off = ar.off

    if "B" in phases:
        ar.off = base_off
        bB = [ar.f32(8, 512) for _ in range(2)]
        nb = 0
        for hq in range(2):
            load_fm(kT, 5 + hq, "kT")
            load_fm(qT, 3 + hq, "qT")
            load_v(1 + hq)
            sc_ = HD ** -0.5
            cur_pat = None
            for blk, (q0, N) in enumerate(qblocks):
                pat = bpat_of_block[blk]
                if pat != cur_pat:
                    bi_ = nb % 2
                    nb += 1
                    for i in range(8):
                        S.dma("act", bB[bi_][:, i, :], biasB[hq, pat, i], writes=["bB%d" % bi_])
                    cur_pat = pat
                    cur_b = bi_
                r_lo = b_rlo_of_block[blk]
                keys = [(r_lo // 2 + i, bB[cur_b][:, i, :], "bB%d" % cur_b) for i in range(8)] + ctx_keys
                oi, li = 2 + (state["o"] % 2), 4 + (state["o"] % 2)
                attn_block(qT[:, q0:q0 + N], N, keys, sc_, oi, li)
                ob_ = osb[state["o"] % 2]
                ko = "osb%d" % (state["o"] % 2)
                finish_softmax(N, oi, li, None, ob_, ko)
                store_o(2 + hq, q0, N, ob_, ko)
            if with_ctx:
                N = CTX
                oi, li = 2 + (state["o"] % 2), 4 + (state["o"] % 2)
                attn_block(qT[:, S_:S_ + N], N, ctx_keys, sc_, oi, li)
                ob_ = osb[state["o"] % 2]
                ko = "osb%d" % (state["o"] % 2)
                finish_softmax(N, oi, li, None, ob_, ko)
                store_o(2 + hq, S_, N, ob_, ko)
        S.barrier()

    if "C" in phases:
        ar.off = base_off
        lt = ar.f32(4, 64)
        S.dma("sp", lt, lamc, writes=["lt"])
        S.op("dve", lambda e: e.tensor_tensor(lt[:, 0, :], lt[:, 0, :], lt[:, 1, :], ALU.mult), reads=["lt"], writes=["lt"])
        S.op("dve", lambda e: e.tensor_tensor(lt[:, 2, :], lt[:, 2, :], lt[:, 3, :], ALU.mult), reads=["lt"], writes=["lt"])
        S.op("dve", lambda e: e.tensor_reduce(out=sm[:, 8:9], in_=lt[:, 0, :], axis=AX.X, op=ALU.add), reads=["lt"], writes=["sm_lam"])
        S.op("dve", lambda e: e.tensor_reduce(out=sm[:, 9:10], in_=lt[:, 2, :], axis=AX.X, op=ALU.add), reads=["lt"], writes=["sm_lam"])
        S.op("act", lambda e: e.activation(out=sm[:, 10:12], in_=sm[:, 8:10], func=AF.Exp), reads=["sm_lam"], writes=["sm_lam"])
        S.op("dve", lambda e: e.tensor_tensor(sm[:, 12:13], sm[:, 11:12], sm[:, 10:11], ALU.subtract), reads=["sm_lam"], writes=["sm_lam"])
        S.op("dve", lambda e: e.tensor_scalar(sm[:, 12:13], sm[:, 12:13], -lam_init, None, ALU.add), reads=["sm_lam"], writes=["sm_lam"])
        S.op("dve", lambda e: e.tensor_scalar(sm[:, 13:14], gn[:, 6:7], 1.0 - lam_init, None, ALU.mult), reads=["gn"], writes=["sm_sub"])
        sqc = ar.bf(512)
        sc_ = 64 ** -0.5
        all_keys = [(i, None, None) for i in range(NKT)]

        def c_block(q0, N, keys, slot):
            attn_block2(qT[:, q0:q0 + N], N, keys, sc_)
            finish_softmax(N, 2, 4, None, on[0], "on0")
            finish_softmax(N, 3, 5, None, on[1], "on1")
            S.op("dve", lambda e: e.scalar_tensor_tensor(on[2][:, 0:N], on[1][:, 0:N], sm[:, 12:13], on[0][:, 0:N], ALU.mult, ALU.add),
                 reads=["on0", "on1", "sm_lam"], writes=["on2"])
            S.op("act", lambda e: e.activation(out=sqc[:, 0:N], in_=on[2][:, 0:N], func=AF.Square), reads=["on2"], writes=["sqc"])
            S.op("pe", lambda e: e.matmul(ps[6][:, 0:N], ONES, sqc[:, 0:N], start=True, stop=True), reads=["sqc", "cm"], writes=[PK(6)])
            rstd_from(ps[6], N, 128, tf[0], PK(6), "tf0")
            ob_ = osb[state["o"] % 2]
            ko = "osb%d" % (state["o"] % 2)
            S.op("dve", lambda e: e.scalar_tensor_tensor(ob_[:, 0:N], on[2][:, 0:N], sm[:, 13:14], tf[0][:, 0:N], ALU.mult, ALU.mult),
                 reads=["on2", "sm_sub", "tf0"], writes=[ko])
            store_o(slot, q0, N, ob_, ko)

        for hq in range(2):
            load_fm(kT, 9 + hq, "kT")
            load_fm(qT, 7 + hq, "qT")
            load_v(3 + hq)
            for (q0, N) in qblocks:
                c_block(q0, N, all_keys, 4 + hq)
            if with_ctx:
                c_block(S_, CTX, ctx_keys, 4 + hq)
        S.barrier()

    if "D" in phases:
        ar.off = base_off
        gT = ar.bf(NT)
        ktm = ar.bf(NKT, 128)
        osum = ar.f32(NT)
        osum2 = ar.f32(NT)
        dtb = ar.f32(6, 128)
        for t_ in range(6):
            S.dma("sp", dtb[:, t_, :], dtab[t_], writes=["dtb"])
        dcl = ar.f32(2)
        S.dma("sp", dcl, dcol, writes=["dcl"])
        lg = ar.f32(4)
        S.dma("sp", lg, lgd, writes=["lg"])
        intra = [ar.bf(128) for _ in range(2)]
        qdec = [ar.f32(128) for _ in range(2)]
        kdec = ar.f32(4)
        tfd = ar.f32(128)
        PTd = [ar.bf(128) for _ in range(2)]
        qd = [ar.bf(128) for _ in range(2)]
        kdk = [ar.bf(128) for _ in range(2)]
        Sf = ar.f32(128)
        Sb = [ar.bf(128) for _ in range(2)]
        sqd = ar.bf(512)
        ctx_chunks_f = [NLT + i for i in range(CTX // 128)]
        lat_chunks_f = list(range(NLT))
        for hq in range(2):
            load_fm(kT, 13 + hq, "kT")
            load_fm(qT, 11 + hq, "qT")
            load_fm(gT, 15 + hq, "gT")
            load_v(5 + hq)
            for c in range(0, NKT, 4):
                n = min(4, NKT - c)
                S.dma("act", ktm[:, c:c + n, :], VT[7 + hq][c * 128:(c + n) * 128, :].rearrange("(c p) d -> p c d", p=128), reads=VK(7 + hq), writes=["ktm"])
            for dr in range(2):
                lgc = lg[:, dr * 2 + hq:dr * 2 + hq + 1]
                S.op("act", lambda e, dr=dr, lgc=lgc: e.activation(out=tfd, in_=dtb[:, 2 * dr, :], func=AF.Exp, scale=lgc), reads=["dtb", "lg"], writes=["tfd"])
                S.op("dve", lambda e, dr=dr: e.tensor_tensor(intra[dr], tfd, dtb[:, 2 * dr + 1, :], ALU.mult), reads=["tfd", "dtb"], writes=["intra%d" % dr])
                S.op("act", lambda e, dr=dr, lgc=lgc: e.activation(out=qdec[dr], in_=dtb[:, 4 + dr, :], func=AF.Exp, scale=lgc), reads=["dtb", "lg"], writes=["qdec%d" % dr])
                S.op("act", lambda e, dr=dr, lgc=lgc: e.activation(out=kdec[:, dr:dr + 1], in_=dcl[:, dr:dr + 1], func=AF.Exp, scale=lgc), reads=["dcl", "lg"], writes=["kdec"])
                S.op("act", lambda e, dr=dr, lgc=lgc: e.activation(out=kdec[:, 2 + dr:3 + dr], in_=lgc, func=AF.Exp, scale=128.0), reads=["lg"], writes=["kdec"])
                order = (ctx_chunks_f + lat_chunks_f) if dr == 0 else (ctx_chunks_f[::-1] + lat_chunks_f[::-1])
                S.op("pool", lambda e: e.memset(Sf, 0.0), writes=["Sf"])
                S.op("pool", lambda e: e.memset(Sb[0], 0.0), writes=["Sb0"])
                sbi = 0
                for ci, ch in enumerate(order):
                    t0 = ch * 128
                    is_ctx = ch >= NLT
                    want_out = (not is_ctx) or with_ctx
                    i2 = ci % 2
                    if want_out:
                        S.op("pe", lambda e, t0=t0: e.matmul(ps[0][:, 0:128], kT[:, t0:t0 + 128], qT[:, t0:t0 + 128], start=True, stop=True),
                             reads=["kT", "qT"], writes=[PK(0)])
                        S.op("dve", lambda e, i2=i2, dr=dr: e.tensor_tensor(PTd[i2], ps[0][:, 0:128], intra[dr], ALU.mult),
                             reads=[PK(0), "intra%d" % dr], writes=["PTd%d" % i2])
                        S.op("dve", lambda e, i2=i2, dr=dr, t0=t0: e.tensor_tensor(qd[i2], qT[:, t0:t0 + 128], qdec[dr], ALU.mult),
                             reads=["qT", "qdec%d" % dr], writes=["qd%d" % i2])
                        S.op("pe", lambda e, ch=ch, i2=i2: e.matmul(ps[2][:, 0:128], vv[:, ch, :], PTd[i2], start=True, stop=False),
                             reads=["vv", "PTd%d" % i2], writes=[PK(2)])
                        S.op("pe", lambda e, i2=i2, sbi=sbi: e.matmul(ps[2][:, 0:128], Sb[sbi], qd[i2], start=False, stop=True),
                             reads=["Sb%d" % sbi, "qd%d" % i2], writes=[PK(2)])
                        if dr == 0:
                            S.op("act", lambda e, t0=t0: e.activation(out=osum[:, t0:t0 + 128], in_=ps[2][:, 0:128], func=AF.Copy), reads=[PK(2)], writes=["osum"])
                        else:
                            S.op("act", lambda e, t0=t0: e.activation(out=osum2[:, t0:t0 + 128], in_=ps[2][:, 0:128], func=AF.Copy), reads=[PK(2)], writes=["osum2"])
                    if ci < len(order) - 1:
                        S.op("dve", lambda e, i2=i2, ch=ch, dr=dr: e.tensor_scalar(kdk[i2], ktm[:, ch, :], kdec[:, dr:dr + 1], None, ALU.mult),
                             reads=["ktm", "kdec"], writes=["kdk%d" % i2])
                        S.op("pe", lambda e, i2=i2, ch=ch: e.matmul(ps[4][:, 0:128], kdk[i2], vv[:, ch, :], start=True, stop=True),
                             reads=["kdk%d" % i2, "vv"], writes=[PK(4)])
                        S.op("dve", lambda e, dr=dr: e.scalar_tensor_tensor(Sf, Sf, kdec[:, 2 + dr:3 + dr], ps[4][:, 0:128], ALU.mult, ALU.add),
                             reads=["Sf", "kdec", PK(4)], writes=["Sf"])
                        sbi = 1 - sbi
                        S.op("act", lambda e, sbi=sbi: e.activation(out=Sb[sbi], in_=Sf, func=AF.Copy), reads=["Sf"], writes=["Sb%d" % sbi])
            if _dbg and hq == 0:
                dbg = cx.dout("dbg", [2, 128, NT], F32)
                S.dma("sp", dbg[0], osum, reads=["osum"], writes=["dbg"])
                S.dma("sp", dbg[1], osum2, reads=["osum2"], writes=["dbg"])
            fin_blocks = qblocks + ([(S_, CTX)] if with_ctx else [])
            for (q0, N) in fin_blocks:
                S.op("dve", lambda e, q0=q0, N=N: e.tensor_tensor(osum[:, q0:q0 + N], osum[:, q0:q0 + N], osum2[:, q0:q0 + N], ALU.add),
                     reads=["osum", "osum2"], writes=["osum"])
                S.op("act", lambda e, q0=q0, N=N: e.activation(out=sqd[:, 0:N], in_=osum[:, q0:q0 + N], func=AF.Square), reads=["osum"], writes=["sqd"])
                S.op("pe", lambda e, N=N: e.matmul(ps[6][:, 0:N], ONES, sqd[:, 0:N], start=True, stop=True), reads=["sqd", "cm"], writes=[PK(6)])
                rstd_from(ps[6], N, 128, tf[0], PK(6), "tf0")
                S.op("dve", lambda e, q0=q0, N=N: e.tensor_tensor(tf[1][:, 0:N], osum[:, q0:q0 + N], tf[0][:, 0:N], ALU.mult), reads=["osum", "tf0"], writes=["tf1"])
                ob_ = osb[state["o"] % 2]
                ko = "osb%d" % (state["o"] % 2)
                state["o"] += 1
                S.op("pool", lambda e, ob_=ob_, q0=q0, N=N: e.tensor_tensor(ob_[:, 0:N], tf[1][:, 0:N], gT[:, q0:q0 + N], ALU.mult), reads=["tf1", "gT"], writes=[ko])
                store_o(6 + hq, q0, N, ob_, ko)
    if not with_ctx:
        zt = cx.sb("zt", [128, CTX], BF16)
        S.op("pool", lambda e: e.memset(zt, 0.0), writes=["zt"])
        for sl in range(8):
            S.dma("sp", oT[sl * 128:(sl + 1) * 128, S_:S_ + CTX], zt, reads=["zt"], writes=["oT"])
    return cx.finish()


def rope_tables_fm(L, dim, reps):
    t = np.arange(L)
    row = (t // GRID_W).astype(np.float32)
    col = (t % GRID_W).astype(np.float32)
    quarter = dim // 4
    inv = (np.float32(10000.0) ** (-np.arange(quarter, dtype=np.float32) / np.float32(quarter))).astype(np.float32)
    a_r = row[:, None] * inv[None, :]
    a_c = col[:, None] * inv[None, :]
    ang = np.concatenate([a_r, a_r, a_c, a_c], axis=-1)
    cs = np.stack([np.cos(ang).T, np.sin(ang).T]).astype(np.float32)
    return np.ascontiguousarray(np.tile(cs, (1, reps, 1)))


def rot_lhsT(dim, reps):
    R = np.zeros((128, 128), np.float32)
    q = dim // 4
    for r in range(reps):
        o = r * dim
        for m in range(dim):
            blk = m // q
            if blk % 2 == 0:
                R[o + m + q, o + m] = -1.0
            else:
                R[o + m - q, o + m] = 1.0
    return R


def const_rmat():
    ones = np.ones((128, 128), np.float32)
    bones = np.zeros((128, 128), np.float32)
    bones[:64, :64] = 1.0
    bones[64:, 64:] = 1.0
    return np.stack([rot_lhsT(128, 1), rot_lhsT(64, 2), ones, bones])


def const_maskA():
    k = np.arange(128)[:, None]
    q = np.arange(512)[None, :]
    out = np.zeros((6, 128, 512), np.float32)
    for i in range(6):
        rel = (i - 1) * 128 + k - q
        out[i] = np.where(np.abs(rel) <= 128, 0.0, NEG)
    return out


def b_bias_block(rpb_h, r0, r_lo, R):
    qr = r0 + np.arange(8)
    rs = np.clip(qr - 4, 0, R - 8)
    kr = r_lo + np.arange(16)
    cidx = np.arange(64)
    cs = np.clip(cidx - 8, 0, 64 - 16)
    colmask = (cidx[None, :] >= cs[:, None]) & (cidx[None, :] < cs[:, None] + 16)
    rel_c = np.clip(cidx[None, :] - cidx[:, None] + 15, 0, 30)
    out = np.full((16, 64, 8, 64), NEG, np.float32)
    for qi in range(8):
        for ki in range(16):
            dr = kr[ki] - rs[qi]
            if 0 <= dr < 8:
                rel_r = kr[ki] - qr[qi] + 7
                vals = rpb_h[rel_r][rel_c]
                out[ki, :, qi, :] = np.where(colmask, vals, NEG).T
    return out.reshape(8, 128, 512)


def b_patterns(rpb2, S_):
    R = S_ // GRID_W
    nblk = S_ // 512
    pats, pat_of, rlo_of, seen = [], [], [], {}
    for blk in range(nblk):
        r0 = blk * 8
        r_lo = int(np.clip(r0 - 4, 0, R - 16))
        key = (r0 - r_lo, r0 == 0 or r0 < 4, r0 + 8 + 3 > R - 1 + 0 and (R - 8) - (r0 + 7 - 4) < 0 or r0 + 12 > R)
        key = (r0 - r_lo, tuple(np.clip(r0 + np.arange(8) - 4, 0, R - 8) - r0))
        if key not in seen:
            seen[key] = len(pats)
            pats.append(np.stack([b_bias_block(rpb2[h], r0, r_lo, R) for h in range(2)]))
        pat_of.append(seen[key])
        rlo_of.append(r_lo)
    biasB = np.ascontiguousarray(np.stack(pats, axis=1))
    return biasB, pat_of, rlo_of


def const_dtab():
    j = np.arange(128)[:, None].astype(np.float32)
    i = np.arange(128)[None, :].astype(np.float32)
    relF = np.maximum(i - j, 0.0)
    maskF = (i >= j).astype(np.float32)
    relB = np.maximum(j - i, 0.0)
    maskB = (j >= i).astype(np.float32)
    posF = np.broadcast_to(i + 1.0, (128, 128))
    posB = np.broadcast_to(128.0 - i, (128, 128))
    dtab = np.stack([relF, maskF, relB, maskB, posF, posB]).astype(np.float32)
    jj = np.arange(128).astype(np.float32)
    dcol = np.stack([127.0 - jj, jj], axis=1).astype(np.float32)
    return np.ascontiguousarray(dtab), np.ascontiguousarray(dcol)


IN_OFF = dict(Aq=0, Ak=1024, Av=1280, Bq=1536, Bk=2560, Bv=3584, Cq=4608, Ck=5632, Cv=6656, Dq=7680, Dk=8704, Dv=9728, Dg=10752)


def a_weight_cols(j):
    h0, h1, kv = 2 * j, 2 * j + 1, j // 2
    c = lambda g, h: list(range(IN_OFF[g] + h * 128, IN_OFF[g] + (h + 1) * 128))
    fm = (c("Aq", h0) + c("Aq", h1) + c("Ak", kv) + c("Bq", h0) + c("Bq", h1) + c("Bk", h0) + c("Bk", h1) + c("Cq", h0) + c("Cq", h1)
          + c("Ck", h0) + c("Ck", h1) + c("Dq", h0) + c("Dq", h1) + c("Dk", h0) + c("Dk", h1) + c("Dg", h0) + c("Dg", h1))
    tm = c("Av", kv) + c("Bv", h0) + c("Bv", h1) + c("Cv", h0) + c("Cv", h1) + c("Dv", h0) + c("Dv", h1) + c("Dk", h0) + c("Dk", h1)
    return fm, tm


def bc128(v):
    return np.ascontiguousarray(np.broadcast_to(np.asarray(v, np.float32)[None], (128,) + np.asarray(v).shape))


def prep_A(p, l, S_, hT_b):
    import math
    lam_init = 0.8 - 0.6 * math.exp(-0.3 * l)
    ropeA = rope_tables_fm(S_, 128, 1)
    ropeC = rope_tables_fm(S_, 64, 2)
    rmat = const_rmat()
    maskA = const_maskA()
    dtab, dcol = const_dtab()
    ims = []
    bargs = None
    for i in range(NCORES):
        b, j = i // 4, i % 4
        h0, h1 = 2 * j, 2 * j + 1
        fm, tm = a_weight_cols(j)
        w_in = p["w_in"][l]
        gains = np.zeros((128, 8), np.float32)
        gains[:, 0] = p["qk_norm_a"][l][0]
        gains[:, 1] = p["qk_norm_a"][l][1]
        gains[:, 2] = p["qk_norm_b"][l][0]
        gains[:, 3] = p["qk_norm_b"][l][1]
        gains[:, 4] = np.tile(p["qk_norm_c"][l][0], 2)
        gains[:, 5] = np.tile(p["qk_norm_c"][l][1], 2)
        gains[:, 6] = p["subln_c"][l]
        biasB, pat_of, rlo_of = b_patterns(p["rpb_b"][l][[h0, h1]], S_)
        bargs = (biasB.shape[1], pat_of, rlo_of)
        rld = p["ret_log_decay"][l]
        ims.append(dict(hT=hT_b[b], wfm=np.ascontiguousarray(w_in[:, fm]), wtm=np.ascontiguousarray(w_in[:, tm]), gains=gains,
                        ropeA=ropeA, ropeC=ropeC, rmat=rmat, maskA=maskA, biasB=biasB,
                        sinks=bc128(p["sink_a"][l][[h0, h1]]), lamc=bc128(p["lambda_c"][l]), dtab=dtab, dcol=dcol,
                        lgd=bc128(np.array([rld[0, h0], rld[0, h1], rld[1, h0], rld[1, h1]], np.float32))))
    return lam_init, bargs, ims


def build_B(TOKL, TOKC):
    cx = Ctx()
    S = cx.S
    TOK = TOKL + TOKC
    oTo = cx.din("oTo", [D, TOK], BF16)
    x = cx.din("x", [TOK, D], F32)
    wo = cx.din("wo", [D, D], F32)
    g1 = cx.din("g1", [2, D], F32)
    nm = cx.din("nm", [D], F32)
    sc = cx.din("sc", [2, D], F32)
    sh = cx.din("sh", [2, D], F32)
    rt = cx.din("rt", [D, 36], F32)
    cst = cx.din("cst", [128, 160], F32)
    xmid = cx.dout("xmid", [TOK, D], F32)
    h2o = cx.dout("h2", [TOK, D], BF16)
    route = cx.dout("route", [TOK, 4], F32)
    ps = cx.psum()
    PK = lambda i: "ps%d" % i
    GT = 256
    oTg = cx.sb("oTg", [128, 32, GT], BF16)
    wsl = [cx.sb("wsl%d" % i, [128, 32, 256], BF16) for i in range(2)]
    xm = [cx.sb("xm%d" % i, [128, D], F32) for i in range(2)]
    xs = [cx.sb("xs%d" % i, [128, 256], F32) for i in range(2)]
    t2 = [cx.sb("t2%d" % i, [128, 256], F32) for i in range(2)]
    g1b = cx.sb("g1b", [128, D], F32)
    gsb = cx.sb("gsb", [128, D], F32)
    shb = cx.sb("shb", [128, D], F32)
    tmp = cx.sb("tmp", [128, D], F32)
    hbf = cx.sb("hbf", [128, D], BF16)
    h2T = cx.sb("h2T", [128, 32, 128], F32)
    rtt = cx.sb("rtt", [128, 32, 36], F32)
    cs = cx.sb("cs", [128, 160], F32)
    stt = cx.sb("stt", [128, 4], F32)
    sml = cx.sb("sml", [128, 128], F32)
    S.dma("sp", cs, cst, writes=["cs"])
    S.dma("sp", rtt, rt.rearrange("(c p) n -> p c n", p=128), writes=["rtt"])
    ident = cs[:, 0:128]
    iota = cs[:, 128:160]
    wov = wo.rearrange("(c p) n -> p c n", p=128)
    nW = 0
    wobf = cx.dscr("wobf", [D // 256, 128, 32 * 256], BF16)
    groups = [(g * GT, GT, 0) for g in range(TOKL // GT)] + ([(TOKL, TOKC, 1)] if TOKC else [])
    first_group = [True]
    cur_seg = None

    def route_tile(P, r0):
        KS = ["sml"]
        dv = lambda fn_, r=KS, w=KS: S.op("dve", fn_, reads=r, writes=w)
        c = [sml[0:P, 108 + i:109 + i] for i in range(8)]
        lg_, gl, el = sml[0:P, 0:36], sml[0:P, 0:4], sml[0:P, 4:36]
        gsel, pen, elm, oh, prod = sml[0:P, 36:40], sml[0:P, 40:44], sml[0:P, 44:76], sml[0:P, 76:108], sml[0:P, 0:32]
        S.op("dve", lambda e: e.tensor_copy(lg_, ps[6][0:P, 0:36]), reads=[PK(6)], writes=KS)
        dv(lambda e: e.tensor_reduce(out=c[0], in_=gl, axis=AX.X, op=ALU.max))
        dv(lambda e: e.tensor_scalar(gsel, gl, c[0], None, ALU.is_ge))
        dv(lambda e: e.tensor_scalar(c[1], c[0], -1.0, None, ALU.mult))
        S.op("act", lambda e: e.activation(out=pen, in_=gl, func=AF.Exp, bias=c[1], accum_out=c[2]), reads=KS, writes=KS)
        dv(lambda e: e.reciprocal(c[3], c[2]))
        dv(lambda e: e.tensor_scalar(pen, gsel, 1e9, -1e9, ALU.mult, ALU.add))
        for g in range(4):
            dv(lambda e, g=g: e.tensor_scalar(elm[:, g * 8:(g + 1) * 8], el[:, g * 8:(g + 1) * 8], pen[:, g:g + 1], None, ALU.add))
        dv(lambda e: e.tensor_reduce(out=c[4], in_=elm, axis=AX.X, op=ALU.max))
        dv(lambda e: e.tensor_scalar(oh, elm, c[4], None, ALU.is_ge))
        dv(lambda e: e.tensor_tensor(prod, oh, iota[0:P, :], ALU.mult))
        dv(lambda e: e.tensor_reduce(out=stt[0:P, 0:1], in_=prod, axis=AX.X, op=ALU.add), w=["stt", "sml"])
        dv(lambda e: e.scalar_tensor_tensor(elm, oh, -1e9, elm, ALU.mult, ALU.add))
        dv(lambda e: e.tensor_reduce(out=c[5], in_=elm, axis=AX.X, op=ALU.max))
        dv(lambda e: e.tensor_scalar(oh, elm, c[5], None, ALU.is_ge))
        dv(lambda e: e.tensor_tensor(prod, oh, iota[0:P, :], ALU.mult))
        dv(lambda e: e.tensor_reduce(out=stt[0:P, 1:2], in_=prod, axis=AX.X, op=ALU.add), w=["stt", "sml"])
        dv(lambda e: e.tensor_tensor(c[6], c[4], c[5], ALU.subtract))
        S.op("act", lambda e: e.activation(out=c[7], in_=c[6], func=AF.Sigmoid), reads=KS, writes=KS)
        dv(lambda e: e.tensor_tensor(stt[0:P, 2:3], c[7], c[3], ALU.mult), w=["stt", "sml"])
        dv(lambda e: e.tensor_tensor(stt[0:P, 3:4], c[3], stt[0:P, 2:3], ALU.subtract), r=["stt", "sml"], w=["stt"])
        S.dma("sp", route[r0:r0 + P, :], stt[0:P, :], reads=["stt"])

    for (g0, gn, seg) in groups:
        if seg != cur_seg:
            cur_seg = seg
            load_bcast(S, "act", g1b, g1[seg], "g1b")
            load_bcast(S, "act", gsb, sc[seg], "gsb")
            load_bcast(S, "act", shb, sh[seg], "shb")
            load_bcast(S, "act", tmp, nm, "tmpnm", also=["tmp"])
            S.op("dve", lambda e: e.scalar_tensor_tensor(gsb, gsb, 1.0, tmp, ALU.add, ALU.mult), reads=BK4("gsb") + BK4("tmpnm") + ["tmp"], writes=BK4("gsb") + ["tmp"])
        for h in range(8):
            S.dma("sp", oTg[:, h * 4:(h + 1) * 4, 0:gn], oTo.rearrange("(c p) t -> p c t", p=128)[:, h * 4:(h + 1) * 4, g0:g0 + gn], writes=["oTg_%d" % h])
        tiles = [(t0, min(128, gn - t0)) for t0 in range(0, gn, 128)]
        for cg in range(D // 256):
            wi = nW % 2
            nW += 1
            wkeys = ["wsl%d_%d" % (wi, h) for h in range(8)]
            if first_group[0]:
                for h in range(8):
                    S.dma("pool", wsl[wi][:, h * 4:(h + 1) * 4, :], wov[:, h * 4:(h + 1) * 4, cg * 256:(cg + 1) * 256], writes=["wsl%d_%d" % (wi, h)])
                S.dma("act", wobf[cg], wsl[wi].rearrange("p c n -> p (c n)"), reads=wkeys, writes=["wobf%d" % cg])
            else:
                S.dma("sp" if cg % 2 == 0 else "act", wsl[wi].rearrange("p c n -> p (c n)"), wobf[cg], reads=["wobf%d" % cg], writes=wkeys)
            for ti, (t0, P) in enumerate(tiles):
                pb = (cg * 2 + ti) % 4
                for k in range(32):
                    S.op("pe", lambda e, pb=pb, k=k, t0=t0, P=P, wi=wi: e.matmul(ps[pb][0:P, 0:256], oTg[:, k, t0:t0 + P], wsl[wi][:, k, :], start=(k == 0), stop=(k == 31)),
                         reads=["oTg_%d" % (k // 4), "wsl%d_%d" % (wi, k // 4)], writes=[PK(pb)])
                xi = (cg * 2 + ti) % 2
                S.dma("sp", xs[xi][0:P, :], x[g0 + t0:g0 + t0 + P, cg * 256:(cg + 1) * 256], writes=["xs%d" % xi])
                S.op("dve", lambda e, pb=pb, xi=xi, P=P, cg=cg: e.tensor_tensor(t2[xi][0:P, :], ps[pb][0:P, 0:256], g1b[0:P, cg * 256:(cg + 1) * 256], ALU.mult),
                     reads=[PK(pb)] + BK4("g1b"), writes=["t2%d" % xi])
                S.op("pool", lambda e, xi=xi, P=P, cg=cg, ti=ti: e.tensor_tensor(xm[ti][0:P, cg * 256:(cg + 1) * 256], t2[xi][0:P, :], xs[xi][0:P, :], ALU.add),
                     reads=["t2%d" % xi, "xs%d" % xi], writes=["xm%d_%d" % (ti, cg)])
        first_group[0] = False
        for ti, (t0, P) in enumerate(tiles):
            r0 = g0 + t0
            kxm = ["xm%d_%d" % (ti, cg) for cg in range(D // 256)]
            for h in range(2):
                S.dma("act", xmid[r0:r0 + P, h * 2048:(h + 1) * 2048], xm[ti][0:P, h * 2048:(h + 1) * 2048], reads=kxm)
            emit_modnorm(S, xm[ti][0:P, :], P, gsb[0:P, :], shb[0:P, :], tmp[0:P, :], tmp[0:P, :], hbf[0:P, :], stt[0:P, :],
                         kxm, BK4("gsb"), BK4("shb"), ["tmp"], ["tmp"], ["hbf"], ["stt"], eng2="pool")
            S.op("act", lambda e, P=P: e.activation(out=hbf[0:P, :], in_=tmp[0:P, :], func=AF.Copy), reads=["tmp"], writes=["hbf"])
            S.dma("act", h2o[r0:r0 + P, :], hbf[0:P, :], reads=["hbf"])
            for q4 in range(8):
                pb = 4 + q4 % 2
                for s4 in range(4):
                    k = q4 * 4 + s4
                    S.op("pe", lambda e, pb=pb, s4=s4, k=k, P=P: e.transpose(ps[pb][:, s4 * 128:s4 * 128 + P], tmp[0:P, k * 128:(k + 1) * 128], ident[0:P, 0:P]),
                         reads=["tmp", "cs"], writes=[PK(pb)])
                eng = "act" if q4 % 2 == 0 else "dve"
                if eng == "act":
                    S.op("act", lambda e, pb=pb, q4=q4, P=P: e.activation(out=h2T[:, q4 * 4:(q4 + 1) * 4, 0:P], in_=ps[pb].rearrange("p (a b) -> p a b", a=4)[:, :, 0:P], func=AF.Copy),
                         reads=[PK(pb)], writes=["h2T_%d" % q4])
                else:
                    S.op("dve", lambda e, pb=pb, q4=q4, P=P: e.tensor_copy(h2T[:, q4 * 4:(q4 + 1) * 4, 0:P], ps[pb].rearrange("p (a b) -> p a b", a=4)[:, :, 0:P]),
                         reads=[PK(pb)], writes=["h2T_%d" % q4])
            for k in range(32):
                S.op("pe", lambda e, k=k, P=P: e.matmul(ps[6][0:P, 0:36], h2T[:, k, 0:P], rtt[:, k, :], start=(k == 0), stop=(k == 31)),
                     reads=["h2T_%d" % (k // 4), "rtt"], writes=[PK(6)])
            route_tile(P, r0)
    return cx.finish()


def build_C(CAP):
    cx = Ctx()
    S = cx.S
    NE = 4
    hs = cx.din("hs", [NE, D, CAP], BF16)
    wg = cx.din("wg", [NE, D, 512], F32)
    wu = cx.din("wu", [NE, D, 512], F32)
    wd = cx.din("wd", [NE, 512, D], F32)
    y = cx.dout("y", [NE, D, CAP], BF16)
    ps = cx.psum()
    PK = lambda i: "ps%d" % i
    Wg = cx.sb("Wg", [128, 32, 512], BF16)
    Wu = cx.sb("Wu", [128, 32, 512], BF16)
    Wd = cx.sb("Wd", [128, 4, D], BF16)
    hb = [cx.sb("hsb%d" % i, [128, 32, 512], BF16) for i in range(2)]
    act = cx.sb("actT", [128, 4, 512], BF16)
    sa = [cx.sb("sa%d" % i, [128, 512], F32) for i in range(2)]
    ysb = cx.sb("ysb", [128, 32, 512], BF16)
    tiles = [(s0, min(512, CAP - s0)) for s0 in range(0, CAP, 512)]
    nh = 0
    npb = 0
    for e_ in range(NE):
        for h in range(8):
            S.dma("pool", Wg[:, h * 4:(h + 1) * 4, :], wg[e_].rearrange("(c p) n -> p c n", p=128)[:, h * 4:(h + 1) * 4, :], writes=["Wg_%d" % h])
        for h in range(8):
            S.dma("pool", Wu[:, h * 4:(h + 1) * 4, :], wu[e_].rearrange("(c p) n -> p c n", p=128)[:, h * 4:(h + 1) * 4, :], writes=["Wu_%d" % h])
        for h in range(8):
            S.dma("pool", Wd[:, :, h * 512:(h + 1) * 512], wd[e_].rearrange("(c p) n -> p c n", p=128)[:, :, h * 512:(h + 1) * 512], writes=["Wd_%d" % h])
        for (s0, N) in tiles:
            hi = nh % 2
            nh += 1
            for h in range(8):
                S.dma("sp", hb[hi][:, h * 4:(h + 1) * 4, 0:N], hs[e_].rearrange("(c p) s -> p c s", p=128)[:, h * 4:(h + 1) * 4, s0:s0 + N], writes=["hsb%d_%d" % (hi, h)])
            for fc in range(4):
                pa, pu = (npb % 2) * 2, (npb % 2) * 2 + 1
                npb += 1
                for (W_, pk_, wn) in ((Wg, pa, "Wg"), (Wu, pu, "Wu")):
                    for k in range(32):
                        S.op("pe", lambda e, W_=W_, pk_=pk_, k=k, fc=fc, hi=hi, N=N: e.matmul(ps[pk_][:, 0:N], W_[:, k, fc * 128:(fc + 1) * 128], hb[hi][:, k, 0:N],
                                                                                              start=(k == 0), stop=(k == 31)),
                             reads=["%s_%d" % (wn, k // 4), "hsb%d_%d" % (hi, k // 4)], writes=[PK(pk_)])
                si = fc % 2
                S.op("act", lambda e, si=si, pa=pa, N=N: e.activation(out=sa[si][:, 0:N], in_=ps[pa][:, 0:N], func=AF.Silu), reads=[PK(pa)], writes=["sa%d" % si])
                S.op("dve", lambda e, si=si, pu=pu, fc=fc, N=N: e.tensor_tensor(act[:, fc, 0:N], sa[si][:, 0:N], ps[pu][:, 0:N], ALU.mult),
                     reads=["sa%d" % si, PK(pu)], writes=["act_%d" % fc])
            for dc in range(32):
                pb = 4 + dc % 4
                for fc in range(4):
                    S.op("pe", lambda e, pb=pb, fc=fc, dc=dc, N=N: e.matmul(ps[pb][:, 0:N], Wd[:, fc, dc * 128:(dc + 1) * 128], act[:, fc, 0:N], start=(fc == 0), stop=(fc == 3)),
                         reads=["Wd_%d" % (dc // 4), "act_%d" % fc], writes=[PK(pb)])
                if dc % 2 == 0:
                    S.op("act", lambda e, pb=pb, dc=dc, N=N: e.activation(out=ysb[:, dc, 0:N], in_=ps[pb][:, 0:N], func=AF.Copy), reads=[PK(pb)], writes=["ysb_%d" % (dc // 4)])
                else:
                    S.op("dve", lambda e, pb=pb, dc=dc, N=N: e.tensor_copy(ysb[:, dc, 0:N], ps[pb][:, 0:N]), reads=[PK(pb)], writes=["ysb_%d" % (dc // 4)])
            for h in range(8):
                S.dma("act", y[e_].rearrange("(c p) s -> p c s", p=128)[:, h * 4:(h + 1) * 4, s0:s0 + N], ysb[:, h * 4:(h + 1) * 4, 0:N], reads=["ysb_%d" % h])
    return cx.finish()


def _cst_B():
    c = np.zeros((128, 160), np.float32)
    c[:, :128] = np.eye(128, dtype=np.float32)
    c[:, 128:160] = np.arange(32, dtype=np.float32)[None]
    return c


def forward(p, S_, depth, log=print):
    import time
    t00 = time.time()
    x = p["x"]
    B = x.shape[0]
    TOKL = S_ // 4
    TOKCF = CTX // 4
    mod = run_mod(p["c"], p["c_ctx"], p["w_mod"], p["b_mod"])
    log("mod done %.1f" % (time.time() - t00))
    mv = lambda l, k: mod[l][:, k * D:(k + 1) * D]
    seg2 = lambda l, k, b: np.ascontiguousarray(np.stack([mv(l, k)[b], mv(l, k)[2]]))
    xs = []
    for i in range(NCORES):
        b, j = i // 4, i % 4
        xs.append(np.ascontiguousarray(np.concatenate([x[b, j * TOKL:(j + 1) * TOKL], p["ctx"][b, j * TOKCF:(j + 1) * TOKCF]], 0)))
    ncD0 = build_D(TOKL, TOKCF, False, True)
    res = _run(ncD0, [dict(x=xs[i], nm=p["norm_mix"][0], sc=seg2(0, 1, i // 4), sh=seg2(0, 0, i // 4)) for i in range(NCORES)])
    hs_tok = [r["h"] for r in res]
    log("norm0 done %.1f" % (time.time() - t00))
    cstB = _cst_B()
    for l in range(depth):
        with_ctx = l < depth - 1
        TOKC = TOKCF if with_ctx else 0
        hT_b = []
        for b in range(B):
            lat = np.concatenate([hs_tok[b * 4 + j][:TOKL] for j in range(4)], 0)
            cxt = np.concatenate([hs_tok[b * 4 + j][TOKL:TOKL + TOKCF] for j in range(4)], 0)
            hT_b.append(np.ascontiguousarray(np.concatenate([lat, cxt], 0).T))
        lam_init, bargs, imsA = prep_A(p, l, S_, hT_b)
        ncA = build_A(S_, with_ctx, lam_init, *bargs)
        resA = _run(ncA, imsA)
        log("L%d A done %.1f" % (l, time.time() - t00))
        imsB = []
        for i in range(NCORES):
            b, j = i // 4, i % 4
            full = np.empty((D, TOKL + TOKC), NPBF)
            cols = list(range(j * TOKL, (j + 1) * TOKL)) + ([S_ + j * TOKCF + t for t in range(TOKCF)] if with_ctx else [])
            for jj in range(4):
                o = resA[b * 4 + jj]["oT"]
                for g in range(4):
                    for hh in range(2):
                        r0 = g * 1024 + (2 * jj + hh) * 128
                        full[r0:r0 + 128] = o[(2 * g + hh) * 128:(2 * g + hh + 1) * 128][:, cols]
            xi = xs[i][:TOKL + TOKC]
            imsB.append(dict(oTo=full, x=np.ascontiguousarray(xi), wo=p["w_out"][l], g1=seg2(l, 2, b), nm=p["norm_ffn"][l], sc=seg2(l, 4, b), sh=seg2(l, 3, b),
                             rt=np.ascontiguousarray(np.concatenate([p["router_group"][l], p["router_expert"][l]], 1)), cst=cstB))
        ncB = build_B(TOKL, TOKC)
        resB = _run(ncB, imsB)
        log("L%d B done %.1f" % (l, time.time() - t00))
        TOK = TOKL + TOKC
        h2 = np.concatenate([r["h2"] for r in resB], 0)
        route = np.concatenate([r["route"] for r in resB], 0)
        eid = np.rint(route[:, 0:2]).astype(np.int64)
        eid = np.clip(eid, 0, 31)
        T = h2.shape[0]
        flat_e = eid.reshape(-1)
        order = np.argsort(flat_e, kind="stable")
        counts = np.bincount(flat_e, minlength=32)
        CAP = int(max(128, -(-counts.max() // 128) * 128))
        starts = np.cumsum(counts) - counts
        hsE = np.zeros((32, CAP, D), NPBF)
        slot_of = np.empty(2 * T, np.int64)
        for e_ in range(32):
            idx = order[starts[e_]:starts[e_] + counts[e_]]
            hsE[e_, :counts[e_]] = h2[idx // 2]
            slot_of[idx] = np.arange(counts[e_])
        imsC = []
        for i in range(NCORES):
            sl = slice(4 * i, 4 * i + 4)
            imsC.append(dict(hs=np.ascontiguousarray(hsE[sl].transpose(0, 2, 1)), wg=p["w_gate"][l][sl], wu=p["w_up"][l][sl], wd=p["w_down"][l][sl]))
        ncC = build_C(CAP)
        resC = _run(ncC, imsC)
        log("L%d C done (CAP %d) %.1f" % (l, CAP, time.time() - t00))
        yE = np.concatenate([r["y"].transpose(0, 2, 1) for r in resC], 0)
        ysel = yE[flat_e, slot_of].reshape(T, 2, D)
        do_norm = l < depth - 1
        imsD = []
        for i in range(NCORES):
            b = i // 4
            sl = slice(i * TOK, (i + 1) * TOK)
            dd = dict(x=resB[i]["xmid"], ya=np.ascontiguousarray(ysel[sl, 0]), yb=np.ascontiguousarray(ysel[sl, 1]),
                      wts=np.ascontiguousarray(route[sl, 2:4]), g2=seg2(l, 5, b))
            if do_norm:
                dd.update(nm=p["norm_mix"][l + 1], sc=seg2(l + 1, 1, b), sh=seg2(l + 1, 0, b))
            imsD.append(dd)
        ncDl = build_D(TOKL, TOKC, True, do_norm)
        resD = _run(ncDl, imsD)
        log("L%d D done %.1f" % (l, time.time() - t00))
        xs = [r["xo"] for r in resD]
        if do_norm:
            hs_tok = [r["h"] for r in resD]
    out = np.empty((B, S_, D), np.float32)
    for i in range(NCORES):
        b, j = i // 4, i % 4
        out[b, j * TOKL:(j + 1) * TOKL] = xs[i][:TOKL]
    return out


def kernel(**inputs):
    p = {k: np.asarray(v) for k, v in inputs.items()}
    return forward(p, p["x"].shape[1], DEPTH, log=lambda *a: print("[kernel]", *a, flush=True))
```
